# Optimizing a Trainium2 kernel written in Bass

```python
import math
import jax, jax.numpy as jnp
from jax import lax
import numpy as np

D_MODEL = 1024
BATCH = 4
SEQ = 4096
DEPTH = 1

MEM_LEN = 256
HEAD_DIM = 64
CONV_CH = D_MODEL // 2
CONV_WIDTH = 31
ATT_HEADS = (D_MODEL // 2) // HEAD_DIM
KV_HEADS = 2
GQA_GROUP = ATT_HEADS // KV_HEADS
WINDOW = 128
BLOCK = 128
REL_BUCKETS = 32
REL_MAX_DIST = 128
Q_COLS = ATT_HEADS * HEAD_DIM
KV_COLS = KV_HEADS * HEAD_DIM
MIX_WIDTH = CONV_CH + Q_COLS
IN_COLS = 2 * CONV_CH + Q_COLS + 2 * KV_COLS
X_HEADS = 4
X_HEAD_DIM = D_MODEL // X_HEADS
N_EXPERTS = 64
TOP_K = 8
N_GROUPS = 8
TOPK_GROUPS = 4
EXPERT_FF = D_MODEL // 4
SHARED_FF = EXPERT_FF
ROUTED_SCALE = 2.5
EXPERT_BLOCK = 256
ALPHA = (2 * DEPTH) ** 0.25
BETA = (8 * DEPTH) ** -0.25
LN_EPS = 1e-5
NEG_INF = -1e30

kernel_name = 'hymba_conformer_swa_sink_moe_deepnorm'


def layer_norm(x, g, b):
    xf = x.astype(jnp.float32)
    mu = jnp.mean(xf, axis=-1, keepdims=True)
    var = jnp.mean(jnp.square(xf - mu), axis=-1, keepdims=True)
    return ((xf - mu) * lax.rsqrt(var + LN_EPS) * g + b).astype(x.dtype)


def t5_bucket(dist):
    n = jnp.maximum(dist, 0)
    exact = REL_BUCKETS // 2
    large = exact + (jnp.log(jnp.maximum(n, 1).astype(jnp.float32) / exact)
                     / math.log(REL_MAX_DIST / exact) * (REL_BUCKETS - exact)).astype(jnp.int32)
    large = jnp.minimum(large, REL_BUCKETS - 1)
    return jnp.where(n < exact, n, large)


def band_bias_and_mask(rel_bias, seq):
    n_blocks = seq // BLOCK
    qi = jnp.arange(BLOCK)[:, None]
    kj = jnp.arange(2 * BLOCK)[None, :]
    dist = qi + BLOCK - kj
    bias = rel_bias[t5_bucket(dist)].astype(jnp.float32)
    bias = jnp.transpose(bias, (2, 0, 1)).reshape(KV_HEADS, GQA_GROUP, BLOCK, 2 * BLOCK)
    key_pos = jnp.arange(n_blocks)[:, None, None] * BLOCK - BLOCK + kj[None]
    mask = (dist >= 0) & (dist < WINDOW) & (key_pos >= 0)
    return bias, mask


def sliding_window_sink_attention(q, k, v, sinks, bias, mask):
    b, s = q.shape[:2]
    nb = s // BLOCK
    qb = q.reshape(b, nb, BLOCK, KV_HEADS, GQA_GROUP, HEAD_DIM)

    def band(t):
        prev = jnp.pad(t, ((0, 0), (BLOCK, 0), (0, 0), (0, 0)))[:, :s]
        prev = prev.reshape(b, nb, BLOCK, KV_HEADS, HEAD_DIM)
        cur = t.reshape(b, nb, BLOCK, KV_HEADS, HEAD_DIM)
        return jnp.concatenate([prev, cur], axis=2)

    kb, vb = band(k), band(v)
    logits = jnp.einsum('bnqhgd,bnkhd->bnhgqk', qb, kb).astype(jnp.float32) * (HEAD_DIM ** -0.5)
    logits = jnp.where(mask[None, :, None, None], logits + bias, NEG_INF)
    sink = jnp.broadcast_to(sinks.astype(jnp.float32).reshape(KV_HEADS, GQA_GROUP, 1, 1),
                            logits.shape[:-1] + (1,))
    probs = jax.nn.softmax(jnp.concatenate([logits, sink], axis=-1), axis=-1)[..., :-1]
    out = jnp.einsum('bnhgqk,bnkhd->bnqhgd', probs.astype(vb.dtype), vb)
    return out.reshape(b, s, Q_COLS)


def conformer_conv_group(a, g, conv_w, conv_b, ln_g, ln_b):
    u = a * jax.nn.sigmoid(g)
    u = jnp.pad(u, ((0, 0), (CONV_WIDTH - 1, 0), (0, 0)))
    y = lax.conv_general_dilated(u, conv_w[:, None, :], window_strides=(1,), padding='VALID',
                                 dimension_numbers=('NWC', 'WIO', 'NWC'),
                                 feature_group_count=CONV_CH) + conv_b
    y = layer_norm(y, ln_g, ln_b)
    return y * jax.nn.sigmoid(y)


def hybrid_mixer(x, w_in, b_in, conv_w, conv_b, conv_ln_g, conv_ln_b, sinks, bias, mask, w_out, b_out):
    b, s, _ = x.shape
    proj = x @ w_in + b_in
    cuts = [CONV_CH, 2 * CONV_CH, 2 * CONV_CH + Q_COLS, 2 * CONV_CH + Q_COLS + KV_COLS]
    a, g, q, k, v = jnp.split(proj, cuts, axis=-1)
    conv_out = conformer_conv_group(a, g, conv_w, conv_b, conv_ln_g, conv_ln_b)
    attn_out = sliding_window_sink_attention(
        q.reshape(b, s, KV_HEADS, GQA_GROUP, HEAD_DIM),
        k.reshape(b, s, KV_HEADS, HEAD_DIM),
        v.reshape(b, s, KV_HEADS, HEAD_DIM), sinks, bias, mask)
    return jnp.concatenate([conv_out, attn_out], axis=-1) @ w_out + b_out


def memory_cross_attention(x, mem, wq, wkv, wo):
    b, s, _ = x.shape
    q = (x @ wq).reshape(b, s, X_HEADS, X_HEAD_DIM)
    k, v = jnp.split(mem @ wkv, 2, axis=-1)
    k = k.reshape(b, -1, X_HEADS, X_HEAD_DIM)
    v = v.reshape(b, -1, X_HEADS, X_HEAD_DIM)
    logits = jnp.einsum('bshd,bmhd->bhsm', q, k).astype(jnp.float32) * (X_HEAD_DIM ** -0.5)
    probs = jax.nn.softmax(logits, axis=-1).astype(v.dtype)
    out = jnp.einsum('bhsm,bmhd->bshd', probs, v).reshape(b, s, D_MODEL)
    return out @ wo


def route(xt, router_w, router_b):
    t = xt.shape[0]
    scores = jax.nn.sigmoid((xt @ router_w).astype(jnp.float32))
    choice = scores + router_b.astype(jnp.float32)
    grp = choice.reshape(t, N_GROUPS, N_EXPERTS // N_GROUPS)
    grp_score = jnp.sum(lax.top_k(grp, 2)[0], axis=-1)
    _, grp_idx = lax.top_k(grp_score, TOPK_GROUPS)
    grp_keep = jnp.any(grp_idx[:, :, None] == jnp.arange(N_GROUPS)[None, None, :], axis=1)
    keep = jnp.repeat(grp_keep, N_EXPERTS // N_GROUPS, axis=1)
    _, idx = lax.top_k(jnp.where(keep, choice, -jnp.inf), TOP_K)
    w = jnp.take_along_axis(scores, idx, axis=1)
    w = w / jnp.sum(w, axis=-1, keepdims=True) * ROUTED_SCALE
    return idx, w


def moe_ffn(h, router_w, router_b, exp_gate, exp_up, exp_down, sh_gate, sh_up, sh_down):
    b, s, d = h.shape
    xt = h.reshape(-1, d)
    t = xt.shape[0]
    idx, gate = route(xt, router_w, router_b)
    n_assign = t * TOP_K
    flat_e = idx.reshape(-1)
    flat_tok = jnp.repeat(jnp.arange(t, dtype=jnp.int32), TOP_K)
    flat_w = gate.reshape(-1)
    order = jnp.argsort(flat_e)
    e_s, tok_s, w_s = flat_e[order], flat_tok[order], flat_w[order]
    counts = jnp.bincount(flat_e, length=N_EXPERTS)
    seg_start = jnp.cumsum(counts) - counts
    pad_counts = (counts + EXPERT_BLOCK - 1) // EXPERT_BLOCK * EXPERT_BLOCK
    pad_end = jnp.cumsum(pad_counts)
    pad_start = pad_end - pad_counts
    dest = pad_start[e_s] + (jnp.arange(n_assign) - seg_start[e_s])
    n_blocks = -(-n_assign // EXPERT_BLOCK) + N_EXPERTS
    n_rows = n_blocks * EXPERT_BLOCK
    row_tok = jnp.full((n_rows,), t, jnp.int32).at[dest].set(tok_s)
    row_w = jnp.zeros((n_rows,), jnp.float32).at[dest].set(w_s)
    block_e = jnp.minimum(jnp.searchsorted(pad_end, jnp.arange(n_blocks) * EXPERT_BLOCK, side='right'),
                          N_EXPERTS - 1)
    x_pad = jnp.concatenate([xt, jnp.zeros((1, d), xt.dtype)], axis=0)
    xb = x_pad[row_tok].reshape(n_blocks, EXPERT_BLOCK, d)

    def expert_block(args):
        xblk, e = args
        return (jax.nn.silu(xblk @ exp_gate[e]) * (xblk @ exp_up[e])) @ exp_down[e]

    yb = lax.map(expert_block, (xb, block_e)).reshape(n_rows, d)
    routed = jax.ops.segment_sum(yb * row_w[:, None].astype(yb.dtype), row_tok, num_segments=t + 1)[:t]
    shared = (jax.nn.silu(xt @ sh_gate) * (xt @ sh_up)) @ sh_down
    return (routed + shared).reshape(b, s, d)


def setup_inputs(seed: int = 0) -> dict:
    key = jax.random.key(seed)
    ks = jax.random.split(key, 32)
    L, D = DEPTH, D_MODEL
    f32 = jnp.float32

    def nrm(k, shape, scale):
        return jax.random.normal(k, shape, f32) * scale

    def gain(k, shape):
        return 1.0 + 0.05 * jax.random.normal(k, shape, f32)

    return {
        'x': nrm(ks[0], (BATCH, SEQ, D), 1.0),
        'mem': nrm(ks[1], (BATCH, MEM_LEN, D), 1.0),
        'w_in': nrm(ks[2], (L, D, IN_COLS), D ** -0.5),
        'b_in': nrm(ks[3], (L, IN_COLS), 0.01),
        'conv_w': nrm(ks[4], (L, CONV_WIDTH, CONV_CH), CONV_WIDTH ** -0.5),
        'conv_b': nrm(ks[5], (L, CONV_CH), 0.01),
        'conv_ln_g': gain(ks[6], (L, CONV_CH)),
        'conv_ln_b': nrm(ks[7], (L, CONV_CH), 0.01),
        'attn_sinks': nrm(ks[8], (L, ATT_HEADS), 0.5),
        'rel_bias': nrm(ks[9], (REL_BUCKETS, ATT_HEADS), 0.5),
        'w_out': nrm(ks[10], (L, MIX_WIDTH, D), BETA * MIX_WIDTH ** -0.5),
        'b_out': nrm(ks[11], (L, D), 0.01),
        'ln1_g': gain(ks[12], (L, D)),
        'ln1_b': nrm(ks[13], (L, D), 0.01),
        'xq_w': nrm(ks[14], (L, D, D), D ** -0.5),
        'xkv_w': nrm(ks[15], (L, D, 2 * D), D ** -0.5),
        'xo_w': nrm(ks[16], (L, D, D), BETA * D ** -0.5),
        'ln2_g': gain(ks[17], (L, D)),
        'ln2_b': nrm(ks[18], (L, D), 0.01),
        'router_w': nrm(ks[19], (L, D, N_EXPERTS), D ** -0.5),
        'router_b': nrm(ks[20], (L, N_EXPERTS), 0.01),
        'exp_gate': nrm(ks[21], (L, N_EXPERTS, D, EXPERT_FF), D ** -0.5),
        'exp_up': nrm(ks[22], (L, N_EXPERTS, D, EXPERT_FF), D ** -0.5),
        'exp_down': nrm(ks[23], (L, N_EXPERTS, EXPERT_FF, D), BETA * EXPERT_FF ** -0.5),
        'sh_gate': nrm(ks[24], (L, D, SHARED_FF), D ** -0.5),
        'sh_up': nrm(ks[25], (L, D, SHARED_FF), D ** -0.5),
        'sh_down': nrm(ks[26], (L, SHARED_FF, D), BETA * SHARED_FF ** -0.5),
        'ln3_g': gain(ks[27], (L, D)),
        'ln3_b': nrm(ks[28], (L, D), 0.01),
    }


def reference(x, mem, w_in, b_in, conv_w, conv_b, conv_ln_g, conv_ln_b, attn_sinks, rel_bias,
              w_out, b_out, ln1_g, ln1_b, xq_w, xkv_w, xo_w, ln2_g, ln2_b, router_w, router_b,
              exp_gate, exp_up, exp_down, sh_gate, sh_up, sh_down, ln3_g, ln3_b):
    bias, mask = band_bias_and_mask(rel_bias, x.shape[1])
    for l in range(DEPTH):
        mix = hybrid_mixer(x, w_in[l], b_in[l], conv_w[l], conv_b[l], conv_ln_g[l], conv_ln_b[l],
                           attn_sinks[l], bias, mask, w_out[l], b_out[l])
        x = layer_norm(ALPHA * x + mix, ln1_g[l], ln1_b[l])
        cross = memory_cross_attention(x, mem, xq_w[l], xkv_w[l], xo_w[l])
        x = layer_norm(ALPHA * x + cross, ln2_g[l], ln2_b[l])
        ffn = moe_ffn(x, router_w[l], router_b[l], exp_gate[l], exp_up[l], exp_down[l],
                      sh_gate[l], sh_up[l], sh_down[l])
        x = layer_norm(ALPHA * x + ffn, ln3_g[l], ln3_b[l])
    return x
```

```python
import numpy as np
import contextlib
import bisect
import concourse.bass as bass
import concourse.mybir as mybir
from concourse.bass_utils import run_bass_kernel_spmd

F32 = mybir.dt.float32
BF16 = mybir.dt.bfloat16
I32 = mybir.dt.int32
U32 = mybir.dt.uint32
AF = mybir.ActivationFunctionType
ALU = mybir.AluOpType
AX = mybir.AxisListType


class Buf:
    __slots__ = ("w", "r", "name")

    def __init__(self, name=""):
        self.w = {}
        self.r = {}
        self.name = name


class Sched:
    ENG = ("pe", "act", "dve", "pool", "sp")

    def __init__(self, nc, stack, nlanes=32):
        self.nc = nc
        self.sem = {e: stack.enter_context(nc.semaphore("s_" + e)) for e in ("pe", "act", "dve", "pool")}
        self.nl = nlanes
        self.nbg = 64
        self.bg_next = 0
        self.lanes = [stack.enter_context(nc.semaphore("lane%d" % i)) for i in range(nlanes + self.nbg)]
        self.lane_val = [0] * (nlanes + self.nbg)
        self.lane_q = [None] * (nlanes + self.nbg)
        self.lane_rr = 0
        self.sw_rr = 0
        self.hw_rr = 0
        self.q = {e: [] for e in self.ENG}
        self.inc_pos = {e: [] for e in self.ENG}
        self.seen = {e: {} for e in self.ENG}
        self.nops = 0

    def _resolve(self, eng, idx):
        ip = self.inc_pos[eng]
        j = bisect.bisect_left(ip, idx)
        if j == len(ip):
            ent = self.q[eng][idx]
            assert ent[0] == "op"
            ent[2] = True
            ip.append(idx)
        return j + 1

    def _wait(self, eng, key, v):
        if key[0] == "e":
            sem = self.sem[key[1]]
            val = self._resolve(key[1], v)
        else:
            sem = self.lanes[key[1]]
            val = v
        if self.seen[eng].get(key, 0) >= val:
            return
        self.seen[eng][key] = val
        self.q[eng].append(["wait", sem, val])

    def _deps(self, eng, reads, writes):
        deps = {}
        for b in reads:
            for k, v in b.w.items():
                if deps.get(k, -1) < v:
                    deps[k] = v
        for b in writes:
            for d in (b.w, b.r):
                for k, v in d.items():
                    if deps.get(k, -1) < v:
                        deps[k] = v
        for k, v in deps.items():
            if eng == "pe" and k == ("e", "pe"):
                continue
            self._wait(eng, k, v)

    def _mark(self, key, val, reads, writes):
        for b in reads:
            if b.r.get(key, -1) < val:
                b.r[key] = val
        for b in writes:
            b.w = {key: val}
            b.r = {}

    def op(self, eng, fn, reads=(), writes=()):
        self._deps(eng, reads, writes)
        idx = len(self.q[eng])
        self.q[eng].append(["op", fn, False])
        self._mark(("e", eng), idx, reads, writes)
        self.nops += 1

    def dma(self, qeng, fn, reads=(), writes=(), bg=False):
        if bg:
            lane = self.nl + self.bg_next
            self.bg_next += 1
            assert self.bg_next <= self.nbg
        else:
            half = self.nl // 2
            if qeng == "pool":
                lane = self.sw_rr
                self.sw_rr = (lane + 1) % half
            else:
                lane = half + self.hw_rr
                self.hw_rr = (self.hw_rr + 1) % half
        if self.lane_val[lane] > 0:
            self._wait(qeng, ("l", lane), self.lane_val[lane])
        self._deps(qeng, reads, writes)
        self.lane_val[lane] += 16
        self.lane_q[lane] = qeng
        self.q[qeng].append(["dma", fn, lane])
        self._mark(("l", lane), self.lane_val[lane], reads, writes)
        self.nops += 1

    def barrier(self, final=False, skip_q=()):
        last = {}
        for e in ("pe", "act", "dve", "pool"):
            for i in range(len(self.q[e]) - 1, -1, -1):
                if self.q[e][i][0] == "op":
                    last[e] = i
                    break
        for e in self.ENG:
            for f, i in last.items():
                if f != e:
                    self._wait(e, ("e", f), i)
            for l, v in enumerate(self.lane_val):
                if v > 0 and (final or l < self.nl) and self.lane_q[l] not in skip_q:
                    self._wait(e, ("l", l), v)

    def finish(self):
        self.barrier(final=True)

    def flush(self):
        sched = self
        with self.nc.Block() as block:
            for e, deco in (("pe", block.tensor), ("act", block.scalar), ("dve", block.vector),
                            ("pool", block.gpsimd), ("sp", block.sync)):
                entries = self.q[e]

                def body(engine, entries=entries, e=e):
                    for ent in entries:
                        if ent[0] == "wait":
                            engine.wait_ge(ent[1], ent[2])
                        elif ent[0] == "op":
                            ins = ent[1](engine)
                            if ent[2]:
                                ins.then_inc(sched.sem[e], 1)
                        else:
                            ins = ent[1](engine)
                            ins.then_inc(sched.lanes[ent[2]], 16)
                deco(body)


C_CAP = 512
NST = C_CAP // 128
NSLOT = 64 * C_CAP
ALPHA = 2.0 ** 0.25
LN_EPS = 1e-5
ARENA_BYTES = 200 * 1024


def build_program(stage=99):
    nc = bass.Bass("TRN2", target_bir_lowering=False)

    def din(name, shape, dt=F32):
        return nc.dram_tensor(name, list(shape), dt, kind="ExternalInput").ap()

    xT_d = din("xT", [128, 8 * 2176])
    xtok_d = din("xtok", [2048, 1024]); memT_d = din("memT", [1024, 256])
    hv_d = din("hv", [128, 1])
    win_d = din("win", [1024, 1920]); bcols_d = din("bcols", [128, 13]); bv_d = din("bv", [1, 256])
    cw_d = din("cw", [128, 124]); cvec_d = din("cvec", [128, 12])
    sinks_d = din("sinks", [1, 8]); relb_d = din("relb", [32, 8])
    wout_d = din("wout", [1024, 1024]); bout_d = din("bout", [1, 1024]); lnp_d = din("lnp", [6, 1024])
    wq_d = din("wq", [1024, 1024]); wkv_d = din("wkv", [1024, 2048]); wo_d = din("wo", [1024, 1024])
    wr_d = din("wr", [1024, 64]); rb_d = din("rb", [1, 64])
    eg_d = din("eg", [64, 1024, 256]); eu_d = din("eu", [64, 1024, 256]); ed_d = din("ed", [64, 256, 1024])
    sg_d = din("sg", [1024, 256]); su_d = din("su", [1024, 256]); sd_d = din("sd", [256, 1024])
    ident_d = din("ident", [128, 128]); J_d = din("Jm", [128, 128]); ohd_d = din("ohd", [32, 128])
    tri_d = din("tri", [128, 128]); ecol_d = din("ecol", [1, 64])
    out_d = nc.dram_tensor("out", [2048, 1024], F32, kind="ExternalOutput").ap()
    xg_d = nc.dram_tensor("xg_scr", [NSLOT + 1, 1152], BF16).ap()
    ye_d = nc.dram_tensor("ye_scr", [NSLOT + 1, 1024], BF16).ap()
    rrow_t = nc.dram_tensor("rrow_scr", [8, 384], F32)
    rrow_d = rrow_t.ap()

    with contextlib.ExitStack() as st:
        S = Sched(nc, st)
        ARENA = st.enter_context(nc.sbuf_tensor("arena", [128, ARENA_BYTES // 2], BF16))
        PRM = st.enter_context(nc.sbuf_tensor("prm", [128, 192], F32))
        SMALL = st.enter_context(nc.sbuf_tensor("small", [128, 4, 32], F32))
        SLOTS = st.enter_context(nc.sbuf_tensor("slots", [128, 16, 8], I32))
        ZT = st.enter_context(nc.sbuf_tensor("zt", [128, 1152], BF16))
        identb = st.enter_context(nc.sbuf_tensor("identb", [128, 128], BF16))
        identf = st.enter_context(nc.sbuf_tensor("identf", [128, 128], F32))
        Jf = st.enter_context(nc.sbuf_tensor("Jf", [128, 128], F32))
        trib = st.enter_context(nc.sbuf_tensor("trib", [128, 128], BF16))
        onesb = st.enter_context(nc.sbuf_tensor("onesb", [128, 128], BF16))
        onesdiv = st.enter_context(nc.sbuf_tensor("onesdiv", [128, 128], F32))
        cstage = st.enter_context(nc.sbuf_tensor("cstage", [128, 128], F32))
        PS = st.enter_context(nc.psum_tensor("ps", [128, 8, 512], F32))

        def A(off, dt, *shape):
            n = 1
            for s in shape:
                n *= s
            if dt == BF16:
                ap = ARENA[:, off // 2: off // 2 + n]
            else:
                ap = ARENA[:, off // 2: off // 2 + 2 * n].bitcast(dt)
            if len(shape) == 2:
                ap = ap.rearrange("p (a b) -> p a b", a=shape[0])
            elif len(shape) == 3:
                ap = ap.rearrange("p (a b c) -> p a b c", a=shape[0], b=shape[1])
            return ap

        PB = [Buf("psum%d" % i) for i in range(8)]
        pstate = {"rr": 0}

        def nbank():
            b = pstate["rr"]
            pstate["rr"] = (b + 1) % 8
            return b

        def pf(b, n=512):
            return PS[:, b, 0:n]

        def pbf(b):
            return PS[:, b, :].bitcast(BF16)

        def mm(out, lhsT, rhs, start, stop, reads, wbuf):
            S.op("pe", lambda e: e.matmul(out, lhsT=lhsT, rhs=rhs, start=start, stop=stop), reads=reads, writes=[wbuf])

        def castdma(out, in_, writes, reads=()):
            S.dma("pool", lambda e: e.dma_start(out=out, in_=in_), reads=reads, writes=writes)

        def spdma(out, in_, writes, reads=()):
            S.dma("sp", lambda e: e.dma_start(out=out, in_=in_), reads=reads, writes=writes)

        Bprm = Buf("prm"); Bconst = Buf("const"); Bcst = Buf("cstage")
        spdma(PRM[:, 0:13], bcols_d, [Bprm])
        spdma(PRM[:, 13:14], hv_d, [Bprm])
        spdma(PRM[:, 16:28], cvec_d, [Bprm])
        spdma(PRM[:, 32:156], cw_d, [Bprm])
        spdma(PRM[:, 160:168], sinks_d[0:1, :].broadcast_to([128, 8]), [Bprm])
        S.op("dve", lambda e: e.memset(PRM[:, 14:15], LN_EPS), writes=[Bprm])
        S.op("act", lambda e: e.activation(out=PRM[:, 168:176], in_=PRM[:, 160:168], func=AF.Exp), reads=[Bprm], writes=[Bprm])
        spdma(identf[:], ident_d, [Bconst])
        spdma(Jf[:], J_d, [Bconst])
        S.op("dve", lambda e: e.tensor_copy(out=identb[:], in_=identf[:]), reads=[Bconst], writes=[Bconst])
        spdma(cstage[:], tri_d, [Bcst])
        S.op("dve", lambda e: e.tensor_copy(out=trib[:], in_=cstage[:]), reads=[Bcst], writes=[Bconst])
        S.op("dve", lambda e: e.memset(onesb[:], 1.0), writes=[Bconst])
        S.op("dve", lambda e: e.memset(onesdiv[:], 1.0 / 512.0), writes=[Bconst])
        b_a = lambda fc: PRM[:, fc:fc + 1]
        b_g = lambda fc: PRM[:, 4 + fc:5 + fc]
        b_q = lambda fc: PRM[:, 8 + fc:9 + fc]
        b_k = PRM[:, 12:13]
        hv = PRM[:, 13:14]
        eps = PRM[:, 14:15]
        convb = lambda fc: PRM[:, 16 + fc:17 + fc]
        clng = lambda fc: PRM[:, 20 + fc:21 + fc]
        clnb = lambda fc: PRM[:, 24 + fc:25 + fc]
        cwk = lambda fc, k: PRM[:, 32 + fc * 31 + k:33 + fc * 31 + k]

        mixT = A(0, BF16, 8, 2048); BmixC = [Buf("mixc%d" % g) for g in range(4)]; BmixA = [Buf("mixa%d" % n) for n in range(16)]
        QT = A(32768, BF16, 4, 2048); BQ = [Buf("q%d" % g) for g in range(4)]
        KT = A(49152, BF16, 2176); BK = [Buf("k%d" % g) for g in range(5)]
        Vt = A(53504, BF16, 17, 256); BV = [Buf("v%d" % g) for g in range(5)]
        uT = A(62208, BF16, 4, 2176); BU = [Buf("u%d" % g) for g in range(5)]
        SCR = 79616

        win = A(SCR, BF16, 8, 1920); Bwin = Buf("win")
        for kc in range(8):
            castdma(win[:, kc, :], win_d[kc * 128:(kc + 1) * 128, :], [Bwin])
        xTg = [A(SCR + 30720 + i * 8192, BF16, 8, 512) for i in range(2)]; BxTg = [Buf("xtg0"), Buf("xtg1")]
        sig = [A(SCR + 30720 + 16384 + i * 2048, F32, 512) for i in range(2)]; Bsig = [Buf("sig0"), Buf("sig1")]
        bvbc = A(SCR + 30720 + 16384 + 4096, F32, 256); Bbv = Buf("bv")
        spdma(bvbc, bv_d[0:1, :].broadcast_to([128, 256]), [Bbv])
        groups = [(0, 128)] + [(128 + g * 512, 512) for g in range(4)]
        def load_x(gi):
            c0, n = groups[gi]
            castdma(xTg[gi % 2].rearrange("p a b -> p (a b)")[:, 0:8 * n], xT_d[:, 8 * c0:8 * (c0 + n)], [BxTg[gi % 2]])
        load_x(0); load_x(1)
        diag = A(SCR + 53248, BF16, 124, 128); Bdiag = [[Buf("diag%d_%d" % (fc, k)) for k in range(31)] for fc in range(4)]

        def build_diag(fc):
            for k in range(31):
                eng = ("dve", "act")[k % 2]
                if eng == "act":
                    S.op("act", lambda e, fc=fc, k=k: e.activation(out=diag[:, fc * 31 + k, :], in_=identf[:], func=AF.Identity, scale=cwk(fc, k)),
                         reads=[Bconst, Bprm], writes=[Bdiag[fc][k]])
                else:
                    S.op(eng, lambda e, fc=fc, k=k: e.tensor_scalar(out=diag[:, fc * 31 + k, :], in0=identf[:], scalar1=cwk(fc, k), scalar2=None, op0=ALU.mult),
                         reads=[Bconst, Bprm], writes=[Bdiag[fc][k]])
        for gi, (c0, n) in enumerate(groups):
            xb_, bx = xTg[gi % 2], BxTg[gi % 2]
            if n != 512:
                xb_ = xb_.rearrange("p a b -> p (a b)")[:, 0:8 * n].rearrange("p (a b) -> p a b", a=8)
            if gi >= 2:
                load_x(gi)
            for fc in range(4):
                ba, bg = nbank(), nbank()
                for kc in range(8):
                    mm(pf(ba, n), win[:, kc, fc * 128:(fc + 1) * 128], xb_[:, kc, 0:n], kc == 0, kc == 7, [Bwin, bx], PB[ba])
                for kc in range(8):
                    mm(pf(bg, n), win[:, kc, 512 + fc * 128:512 + (fc + 1) * 128], xb_[:, kc, 0:n], kc == 0, kc == 7, [Bwin, bx], PB[bg])
                sg_, bs = sig[fc % 2], Bsig[fc % 2]
                S.op("act", lambda e, sg_=sg_, bg=bg, fc=fc, n=n: e.activation(out=sg_[:, 0:n], in_=pf(bg, n), func=AF.Sigmoid, bias=b_g(fc)),
                     reads=[PB[bg], Bprm], writes=[bs])
                S.op("dve", lambda e, sg_=sg_, ba=ba, fc=fc, n=n, c0=c0: e.scalar_tensor_tensor(
                    out=uT[:, fc, c0:c0 + n], in0=pf(ba, n), scalar=b_a(fc), in1=sg_[:, 0:n], op0=ALU.add, op1=ALU.mult),
                    reads=[PB[ba], bs, Bprm], writes=[BU[gi]])
                if gi == 0:
                    S.op("dve", lambda e, fc=fc: e.tensor_scalar(out=uT[:, fc, 0:128], in0=uT[:, fc, 0:128], scalar1=hv, scalar2=None, op0=ALU.mult),
                         reads=[BU[0], Bprm], writes=[BU[0]])
            if gi > 0:
                t0 = c0 - 128
                for fc in range(4):
                    bq = nbank()
                    for kc in range(8):
                        mm(pf(bq, n), win[:, kc, 1024 + fc * 128:1024 + (fc + 1) * 128], xb_[:, kc, 0:n], kc == 0, kc == 7, [Bwin, bx], PB[bq])
                    S.op("act", lambda e, bq=bq, fc=fc, t0=t0, n=n: e.activation(out=QT[:, fc, t0:t0 + n], in_=pf(bq, n), func=AF.Identity, bias=b_q(fc)),
                         reads=[PB[bq], Bprm], writes=[BQ[gi - 1]])
            bk_ = nbank()
            for kc in range(8):
                mm(pf(bk_, n), win[:, kc, 1536:1664], xb_[:, kc, 0:n], kc == 0, kc == 7, [Bwin, bx], PB[bk_])
            S.op("act", lambda e, bk_=bk_, c0=c0, n=n: e.activation(out=KT[:, c0:c0 + n], in_=pf(bk_, n), func=AF.Identity, bias=b_k),
                 reads=[PB[bk_], Bprm], writes=[BK[gi]])
            if gi < 4:
                build_diag(gi)
            for j in range(n // 128):
                bvv = nbank()
                for kc in range(8):
                    mm(pf(bvv, 256), xb_[:, kc, j * 128:(j + 1) * 128], win[:, kc, 1664:1920], kc == 0, kc == 7, [Bwin, bx], PB[bvv])
                ti = c0 // 128 + j
                S.op("dve", lambda e, bvv=bvv, ti=ti: e.tensor_tensor(out=Vt[:, ti, :], in0=pf(bvv, 256), in1=bvbc, op=ALU.add),
                     reads=[PB[bvv], Bbv], writes=[BV[gi]])
        S.barrier()

        Bzt = Buf("zt"); Bxg = Buf("xg_d")
        S.op("dve", lambda e: e.memset(ZT[:], 0.0), writes=[Bzt])
        def zero_fill(r):
            S.dma("sp", lambda e, r=r: e.dma_start(out=xg_d[r * 512:(r + 1) * 512, :].rearrange("(p a) f -> p a f", a=4),
                                                   in_=ZT[:].unsqueeze(1).broadcast_to([128, 4, 1152])), reads=[Bzt], writes=[Bxg], bg=True)
        yf = A(SCR, F32, 4, 512); Byf = Buf("yf")
        ysq = A(SCR + 8192, F32, 4, 512); Bysq = Buf("ysq")
        stt = [A(SCR + 16384 + i * 2048, F32, 512) for i in range(5)]; Bst = [Buf("st%d" % i) for i in range(5)]
        ctmp = [A(SCR + 16384 + 10240 + i * 2048, F32, 512) for i in range(4)]; Bct = [Buf("ct%d" % i) for i in range(4)]
        for tg in range(4):
            pbk = []
            for fc in range(4):
                zero_fill(tg * 4 + fc)
                b = nbank(); pbk.append(b)
                for k in range(31):
                    c0 = 128 + tg * 512 - 30 + k
                    mm(pf(b), diag[:, fc * 31 + k, :], uT[:, fc, c0:c0 + 512], k == 0, k == 30, [Bdiag[fc][k]] + BU, PB[b])
                S.op("act", lambda e, b=b, fc=fc: e.activation(out=yf[:, fc, :], in_=pf(b), func=AF.Identity, bias=convb(fc)),
                     reads=[PB[b], Bprm], writes=[Byf])
                S.op("act", lambda e, b=b, fc=fc: e.activation(out=ysq[:, fc, :], in_=pf(b), func=AF.Square, bias=convb(fc)),
                     reads=[PB[b], Bprm], writes=[Bysq])
            bm, bs2 = nbank(), nbank()
            for fc in range(4):
                mm(pf(bm), onesdiv[:], yf[:, fc, :], fc == 0, fc == 3, [Bconst, Byf], PB[bm])
            for fc in range(4):
                mm(pf(bs2), onesdiv[:], ysq[:, fc, :], fc == 0, fc == 3, [Bconst, Bysq], PB[bs2])
            mean, m2, var, sd, rstd = stt
            S.op("act", lambda e, bm=bm: e.copy(out=mean, in_=pf(bm)), reads=[PB[bm]], writes=[Bst[0]])
            S.op("dve", lambda e: e.tensor_tensor(out=m2, in0=mean, in1=mean, op=ALU.mult), reads=[Bst[0]], writes=[Bst[1]])
            S.op("dve", lambda e, bs2=bs2: e.scalar_tensor_tensor(out=var, in0=m2, scalar=-1.0, in1=pf(bs2), op0=ALU.mult, op1=ALU.add),
                 reads=[Bst[1], PB[bs2]], writes=[Bst[2]])
            S.op("act", lambda e: e.activation(out=sd, in_=var, func=AF.Sqrt, bias=eps), reads=[Bst[2], Bprm], writes=[Bst[3]])
            S.op("dve", lambda e: e.reciprocal(out=rstd, in_=sd), reads=[Bst[3]], writes=[Bst[4]])
            for fc in range(4):
                cen, bc = ctmp[fc], Bct[fc]
                S.op("dve", lambda e, cen=cen, fc=fc: e.tensor_tensor(out=cen, in0=yf[:, fc, :], in1=mean, op=ALU.subtract),
                     reads=[Byf, Bst[0]], writes=[bc])
                S.op("dve", lambda e, cen=cen: e.tensor_tensor(out=cen, in0=cen, in1=rstd, op=ALU.mult), reads=[bc, Bst[4]], writes=[bc])
                S.op("act", lambda e, cen=cen, fc=fc, tg=tg: e.activation(out=mixT[:, fc, tg * 512:(tg + 1) * 512], in_=cen, func=AF.Silu,
                                                                         scale=clng(fc), bias=clnb(fc)),
                     reads=[bc, Bprm], writes=[BmixC[tg]])
        S.barrier()

        SC2 = 131072
        wout = A(SC2, BF16, 8, 1024); Bwout = Buf("wout")
        wostg = A(SC2 + 61440, F32, 2048); Bwostg = Buf("wostg")
        for c in range(4):
            spdma(wostg.rearrange("p (a b) -> p a b", a=2), wout_d[c * 256:(c + 1) * 256, :].rearrange("(kc p) f -> p kc f", p=128), [Bwostg])
            S.op("act", lambda e, c=c: e.copy(out=wout[:, 2 * c:2 * c + 2, :], in_=wostg.rearrange("p (a b) -> p a b", a=2)), reads=[Bwostg], writes=[Bwout])
        boutb = A(SC2 + 16384, BF16, 1024); Bbout = Buf("bout")
        castdma(boutb[0:1, :], bout_d, [Bbout])
        bexp = A(SCR, BF16, 3, 8, 128); Bbexp = Buf("bexp")
        tilep = [A(SCR + 6144 + i * 4096, F32, 8, 128) for i in range(2)]; Btp = [Buf("tp0"), Buf("tp1")]
        Eb = [A(SCR + 14336 + i * 1024, BF16, 512) for i in range(8)]; BE = [Buf("E%d" % i) for i in range(8)]
        Em = [A(SCR + 22528 + i * 1024, BF16, 512) for i in range(8)]; BEm = [Buf("Em%d" % i) for i in range(8)]
        rden = [A(SCR + 37888 + i * 2048, F32, 512) for i in range(4)]; Brden = [Buf("rden%d" % i) for i in range(4)]
        esrow = A(SCR + 46080, BF16, 2, 512); Besr = Buf("esrow")
        rrow_s = A(SCR + 34816, F32, 384); Brr = Buf("rrow_s"); Brd = Buf("rrow_d")
        ohd_s = A(SCR + 36352, F32, 128); relb_s = A(SCR + 36864, F32, 8); Boh = Buf("ohd")
        spdma(ohd_s[0:32, :], ohd_d, [Boh]); spdma(relb_s[0:32, :], relb_d, [Boh])
        S.op("dve", lambda e: e.memset(rrow_s[0:8, :], 0.0), writes=[Brr])
        bt = nbank()
        mm(PS[0:8, bt, 0:128], relb_s[0:32, :], ohd_s[0:32, :], True, True, [Boh], PB[bt])
        S.op("act", lambda e: e.activation(out=rrow_s[0:8, 128:256], in_=PS[0:8, bt, 0:128], func=AF.Exp), reads=[PB[bt]], writes=[Brr])
        spdma(rrow_d, rrow_s[0:8, :], [Brd], reads=[Brr])
        for part, off in ((1, 1), (0, 129)):
            tp, btp = tilep[part], Btp[part]
            spdma(tp, bass.AP(rrow_t, off, [[1, 128], [384, 8], [1, 128]]), [btp], reads=[Brd])
            for hh in range(2):
                b = nbank()
                mm(pf(b), Jf[:], tp[:, hh * 4:(hh + 1) * 4, :], True, True, [Bconst, btp], PB[b])
                S.op("dve", lambda e, b=b, part=part, hh=hh: e.tensor_copy(out=bexp[:, part, hh * 4:(hh + 1) * 4, :],
                                                                         in_=pf(b).rearrange("p (a b) -> p a b", a=4)),
                     reads=[PB[b]], writes=[Bbexp])
        S.op("dve", lambda e: e.tensor_scalar(out=bexp[:, 2, :, :], in0=bexp[:, 0, :, :], scalar1=hv, scalar2=None, op0=ALU.mult),
             reads=[Bbexp, Bprm], writes=[Bbexp])
        for kv in range(2):
            S.op("dve", lambda e, kv=kv: e.tensor_copy(out=esrow[0:1, kv, :].rearrange("p (a b) -> p a b", a=4),
                                                      in_=PRM[0:1, 168 + kv * 4:172 + kv * 4].unsqueeze(2).broadcast_to([1, 4, 128])),
                 reads=[Bprm], writes=[Besr])
        def a3_S(n, kv):
                zero_fill(16 + n * 2 + kv)
                r0 = kv * 64
                ii = (n % 2) * 2 + kv
                for part in range(2):
                    b = nbank()
                    kc0 = (n + part) * 128
                    mm(pf(b), KT[r0:r0 + 64, kc0:kc0 + 128], QT[r0:r0 + 64, :, n * 128:(n + 1) * 128], True, True, BK + BQ, PB[b])
                    ei = ii * 2 + part
                    S.op("act", lambda e, b=b, ei=ei: e.activation(out=Eb[ei], in_=pf(b), func=AF.Exp, scale=0.125), reads=[PB[b]], writes=[BE[ei]])
                    bsel = (2 if n == 0 else 0) if part == 0 else 1
                    eng = "dve"
                    S.op(eng, lambda e, ei=ei, bsel=bsel, kv=kv: e.tensor_tensor(
                        out=Em[ei], in0=Eb[ei], in1=bexp[:, bsel, kv * 4:(kv + 1) * 4, :].rearrange("p a b -> p (a b)"), op=ALU.mult),
                        reads=[BE[ei], Bbexp], writes=[BEm[ei]])

        def a3_P(n, kv):
                ii = (n % 2) * 2 + kv
                bo, bd = nbank(), nbank()
                for part in range(2):
                    ei = ii * 2 + part
                    mm(pf(bo), Vt[:, n + part, kv * 128:(kv + 1) * 128], Em[ei], part == 0, part == 1, BV + [BEm[ei]], PB[bo])
                for part in range(2):
                    ei = ii * 2 + part
                    mm(pf(bd), onesb[:], Em[ei], part == 0, False, [Bconst, BEm[ei]], PB[bd])
                mm(pf(bd), onesb[0:1, :], esrow[0:1, kv, :], False, True, [Bconst, Besr], PB[bd])
                S.op("act", lambda e, ii=ii, bd=bd: e.activation(out=rden[ii], in_=pf(bd), func=AF.Ln), reads=[PB[bd]], writes=[Brden[ii]])
                S.op("act", lambda e, ii=ii: e.activation(out=rden[ii], in_=rden[ii], func=AF.Exp, scale=-1.0), reads=[Brden[ii]], writes=[Brden[ii]])
                for i in range(2):
                    p0 = 64 * i
                    S.op("dve", lambda e, bo=bo, ii=ii, kv=kv, i=i, p0=p0, n=n: e.tensor_tensor(
                        out=mixT[p0:p0 + 64, 4 + 2 * kv:6 + 2 * kv, n * 128:(n + 1) * 128],
                        in0=PS[p0:p0 + 64, bo, :].rearrange("p (j i q) -> p j i q", j=2, i=2)[:, :, i, :],
                        in1=rden[ii][p0:p0 + 64, :].rearrange("p (j i q) -> p j i q", j=2, i=2)[:, :, i, :], op=ALU.mult),
                        reads=[PB[bo], Brden[ii]], writes=[BmixA[n]])
        a3_list = [(n, kv) for n in range(16) for kv in range(2)]
        a3_S(*a3_list[0])
        for j_, nk in enumerate(a3_list):
            if j_ + 1 < len(a3_list):
                a3_S(*a3_list[j_ + 1])
            a3_P(*nk)
        S.barrier()

        resid = A(32768, F32, 16, 1024); BR = [Buf("res%d" % i) for i in range(16)]
        xs_ = A(98304, BF16, 8, 2048); BX = [Buf("xs%d" % g) for g in range(4)]
        SC2 = 131072
        lnbc = A(SC2 + 28672, F32, 2, 1024); Blnbc = Buf("lnbc")
        xbbs = [A(SC2 + 36864 + j * 2048, BF16, 1024) for j in range(4)]; Bxbbs = [Buf("xbb%d" % j) for j in range(4)]
        rbufs = [A(SC2 + 45056 + j * 4096, F32, 1024) for j in range(4)]; Brbs = [Buf("rbuf%d" % j) for j in range(4)]
        Bsms = [Buf("small%d" % j) for j in range(4)]

        def lockstep(gens):
            gens = list(gens)
            while gens:
                for g_ in list(gens):
                    try:
                        next(g_)
                    except StopIteration:
                        gens.remove(g_)

        def run(g_):
            for _ in g_:
                pass
        lnst = {"nb": 2}

        def load_ln(idx):
            spdma(lnbc[:, 0, :], lnp_d[2 * idx:2 * idx + 1, :].broadcast_to([128, 1024]), [Blnbc])
            spdma(lnbc[:, 1, :], lnp_d[2 * idx + 1:2 * idx + 2, :].broadcast_to([128, 1024]), [Blnbc])

        def ln_tile(i, dst, dstbuf, transpose_to=None, gain_eng="pool", defer=None, src=None, srcbuf=None):
            nb_ = lnst["nb"]
            rbuf, Brb, xbb, Bxbb, Bsm = rbufs[i % nb_], Brbs[i % nb_], xbbs[i % nb_], Bxbbs[i % nb_], Bsms[i % nb_]
            if src is not None:
                rbuf, Brb = src, srcbuf
            SM = SMALL[:, i % nb_, :]
            stats = SM[:, 0:12]; mv = SM[:, 12:14]; sdv = SM[:, 14:15]; rsv = SM[:, 15:16]; nbv = SM[:, 16:17]
            S.op("dve", lambda e: e.bn_stats(out=SM[:, 0:6], in_=rbuf[:, 0:512]), reads=[Brb], writes=[Bsm])
            yield
            S.op("dve", lambda e: e.bn_stats(out=SM[:, 6:12], in_=rbuf[:, 512:1024]), reads=[Brb], writes=[Bsm])
            yield
            S.op("dve", lambda e: e.bn_aggr(out=mv, in_=stats), reads=[Bsm], writes=[Bsm])
            yield
            S.op("act", lambda e: e.activation(out=sdv, in_=SM[:, 13:14], func=AF.Ln, bias=eps), reads=[Bsm, Bprm], writes=[Bsm])
            yield
            S.op("act", lambda e: e.activation(out=rsv, in_=sdv, func=AF.Exp, scale=-0.5), reads=[Bsm], writes=[Bsm])
            yield
            S.op("dve", lambda e: e.scalar_tensor_tensor(out=nbv, in0=SM[:, 12:13], scalar=-1.0, in1=rsv, op0=ALU.mult, op1=ALU.mult),
                 reads=[Bsm], writes=[Bsm])
            yield
            S.op("act", lambda e: e.activation(out=dst, in_=rbuf, func=AF.Identity, scale=rsv, bias=nbv), reads=[Brb, Bsm], writes=[dstbuf])
            yield
            S.op(gain_eng, lambda e: e.tensor_tensor(out=dst, in0=dst, in1=lnbc[:, 0, :], op=ALU.mult), reads=[dstbuf, Blnbc], writes=[dstbuf])
            yield
            S.op(gain_eng, lambda e: e.tensor_tensor(out=dst, in0=dst, in1=lnbc[:, 1, :], op=ALU.add), reads=[dstbuf, Blnbc], writes=[dstbuf])
            yield
            if transpose_to is not None:
                S.op("act", lambda e: e.copy(out=xbb, in_=dst), reads=[dstbuf], writes=[Bxbb])
                yield

                def tr_part():
                    for hb in range(2):
                        b = nbank()
                        for j in range(4):
                            kc = hb * 4 + j
                            mm(PS[:, b, j * 128:(j + 1) * 128], xbb[:, kc * 128:(kc + 1) * 128], identb[:], True, True, [Bxbb, Bconst], PB[b])
                        if hb == 0:
                            S.op("dve", lambda e, b=b, i=i, hb=hb: e.tensor_copy(out=xs_[:, hb * 4:hb * 4 + 4, i * 128:(i + 1) * 128], in_=pf(b).rearrange("p (a b) -> p a b", a=4)),
                                 reads=[PB[b]], writes=[BX[i // 4]])
                        else:
                            S.op("act", lambda e, b=b, i=i, hb=hb: e.copy(out=xs_[:, hb * 4:hb * 4 + 4, i * 128:(i + 1) * 128], in_=pf(b).rearrange("p (a b) -> p a b", a=4)),
                                 reads=[PB[b]], writes=[BX[i // 4]])
                if defer is None:
                    tr_part()
                else:
                    defer.append(tr_part)

        xt = [A(SC2 + 18432 + i * 4096, F32, 1024) for i in range(2)]; Bxt = [Buf("xt0"), Buf("xt1")]
        load_ln(0)
        pend = []

        def flush_pend(keep=0):
            while len(pend) > keep:
                pend.pop(0)()
        lnst["nb"] = 4
        for i0_ in range(0, 16, 2):
            for i in (i0_, i0_ + 1):
                spdma(xt[i % 2], xtok_d[i * 128:(i + 1) * 128, :], [Bxt[i % 2]])
                zero_fill(48 + i)
                for h in range(2):
                    b = nbank()
                    for kc in range(8):
                        rb_ = [Bwout, BmixC[i // 4]] if kc < 4 else [Bwout, BmixA[i]]
                        mm(pf(b), mixT[:, kc, i * 128:(i + 1) * 128], wout[:, kc, h * 512:(h + 1) * 512], kc == 0, False, rb_, PB[b])
                    mm(pf(b), onesb[0:1, :], boutb[0:1, h * 512:(h + 1) * 512], False, True, [Bconst, Bbout], PB[b])
                    S.op("dve", lambda e, b=b, h=h, i=i: e.scalar_tensor_tensor(out=rbufs[i % 4][:, h * 512:(h + 1) * 512], in0=xt[i % 2][:, h * 512:(h + 1) * 512],
                                                                               scalar=ALPHA, in1=pf(b), op0=ALU.mult, op1=ALU.add),
                         reads=[Bxt[i % 2], PB[b]], writes=[Brbs[i % 4]])
            flush_pend(keep=2)
            lockstep([ln_tile(i, resid[:, i, :], BR[i], transpose_to=True, defer=pend, gain_eng="dve") for i in (i0_, i0_ + 1)])
        flush_pend()
        lnst["nb"] = 2
        S.barrier()
        if stage <= 1:
            for i in range(16):
                spdma(out_d[i * 128:(i + 1) * 128, :], resid[:, i, :], [Buf()], reads=[BR[i]])
            S.finish(); S.flush()
            return nc

        wq = A(0, BF16, 8, 1024); Bwq = Buf("wq")
        wo = A(16384, BF16, 8, 1024); Bwo = Buf("wo")
        bstg = [A(SC2 + 36864 + j * 8192, F32, 2048) for j in range(3)]; Bbstg = [Buf("bstg%d" % j) for j in range(3)]
        bst = {"k": 0}

        def load_w_sp(dst, src2d, wbuf):
            for c in range(4):
                k = bst["k"] % 3
                bst["k"] += 1
                spdma(bstg[k].rearrange("p (a b) -> p a b", a=2), src2d[c * 256:(c + 1) * 256, :].rearrange("(kc p) f -> p kc f", p=128), [Bbstg[k]])
                if bst["k"] % 2 == 0:
                    S.op("dve", lambda e, k=k, c=c: e.tensor_copy(out=dst[:, 2 * c:2 * c + 2, :], in_=bstg[k].rearrange("p (a b) -> p a b", a=2)), reads=[Bbstg[k]], writes=[wbuf])
                else:
                    S.op("act", lambda e, k=k, c=c: e.copy(out=dst[:, 2 * c:2 * c + 2, :], in_=bstg[k].rearrange("p (a b) -> p a b", a=2)), reads=[Bbstg[k]], writes=[wbuf])
        KmT = A(SC2, BF16, 8, 256); BKm = Buf("KmT")
        Vm = A(SC2 + 4096, BF16, 2, 1024); BVm = Buf("Vm")
        wkv = A(SC2 + 8192, BF16, 8, 1024); Bwkv = Buf("wkv")
        memT = A(SC2 + 8192 + 16384, BF16, 8, 256); BmemT = Buf("memT")
        k_ = bst["k"] % 3
        bst["k"] += 1
        spdma(bstg[k_].rearrange("p (a b) -> p a b", a=8), memT_d.rearrange("(kc p) t -> p kc t", p=128), [Bbstg[k_]])
        S.op("dve", lambda e, k_=k_: e.tensor_copy(out=memT, in_=bstg[k_].rearrange("p (a b) -> p a b", a=8)), reads=[Bbstg[k_]], writes=[BmemT])
        load_w_sp(wkv, wkv_d[:, 0:1024], Bwkv)
        for fc in range(8):
            b = nbank()
            for kc in range(8):
                mm(pf(b, 256), wkv[:, kc, fc * 128:(fc + 1) * 128], memT[:, kc, :], kc == 0, kc == 7, [Bwkv, BmemT], PB[b])
            S.op("act", lambda e, b=b, fc=fc: e.copy(out=KmT[:, fc, :], in_=pf(b, 256)), reads=[PB[b]], writes=[BKm])
        load_w_sp(wq, wq_d, Bwq)
        load_w_sp(wkv, wkv_d[:, 1024:2048], Bwkv)
        for mt in range(2):
            for h in range(2):
                b = nbank()
                for kc in range(8):
                    mm(pf(b), memT[:, kc, mt * 128:(mt + 1) * 128], wkv[:, kc, h * 512:(h + 1) * 512], kc == 0, kc == 7, [Bwkv, BmemT], PB[b])
                S.op("act", lambda e, b=b, mt=mt, h=h: e.copy(out=Vm[:, mt, h * 512:(h + 1) * 512], in_=pf(b)), reads=[PB[b]], writes=[BVm])
        load_ln(1)
        load_w_sp(wo, wo_d, Bwo)
        S.barrier()
        x2Tfs = [A(SC2 + 45056, F32, 8, 128), A(SC2 + 40960, F32, 8, 128)]; Bx2Ts = [Buf("x2Tf0"), Buf("x2Tf1")]
        NXR = 6
        xrow = [A(SC2 + 49152 + i * 2304, BF16, 1152) for i in range(NXR)]; Bxrow = [Buf("xrow%d" % i) for i in range(NXR)]
        RTs = [A(SC2 + 62976 + (j % 2) * 2816, F32, 11, 64) for j in range(2)]; Brts = [Buf("rt%d" % j) for j in range(2)]
        selall = A(SC2 + 68608, BF16, 16, 64); Bsel = [Buf("sel%d" % i) for i in range(16)]
        slots = SLOTS; Bslot = [Buf("slot%d" % i) for i in range(16)]
        wr = A(SC2 + 70656, F32, 8, 64); Bwr = Buf("wr")
        spdma(wr, wr_d.rearrange("(kc p) f -> p kc f", p=128), [Bwr])
        rbbc = A(SC2 + 72704, F32, 64); ecol = A(SC2 + 72960, F32, 64); Brc = Buf("rbec")
        spdma(rbbc, rb_d[0:1, :].broadcast_to([128, 64]), [Brc])
        spdma(ecol, ecol_d[0:1, :].broadcast_to([128, 64]), [Brc])
        BIG = 1.0e9
        def c1_tile(i, part):
            RT, Brt = RTs[i % 2], Brts[i % 2]
            x2Tf, Bx2T = x2Tfs[i % 2], Bx2Ts[i % 2]
            sc, ch, eq, c2, w_, mc, key, t1, selc = (RT[:, j, :] for j in range(9))
            g8 = lambda j, RT=RT: RT[:, 9, j * 8:(j + 1) * 8]
            ws1 = RT[:, 10, 0:1]; rs1 = RT[:, 10, 1:2]
            xr, bxr = xrow[i % NXR], Bxrow[i % NXR]
            gwv = xr[:, 1024:1152].bitcast(F32)
            V3 = lambda ap: ap.rearrange("p (a b) -> p a b", a=8)
            R = [Brt]
            if part == "A":
                x2Tf, Bx2T = x2Tfs[i % 2], Bx2Ts[i % 2]
                b0, b1 = nbank(), nbank()
                for kc in range(8):
                    bb = b0 if kc < 4 else b1
                    S.op("pe", lambda e, bb=bb, kc=kc, i=i: e.transpose(out=PS[:, bb, (kc % 4) * 128:(kc % 4 + 1) * 128], in_=resid[:, i, kc * 128:(kc + 1) * 128], identity=identf[:]),
                         reads=[BR[i], Bconst], writes=[PB[bb]])
                    yield
                S.op("act", lambda e, b0=b0: e.copy(out=x2Tf[:, 0:4, :], in_=pf(b0).rearrange("p (a b) -> p a b", a=4)), reads=[PB[b0]], writes=[Bx2T])
                yield
                S.op("dve", lambda e, b1=b1: e.tensor_copy(out=x2Tf[:, 4:8, :], in_=pf(b1).rearrange("p (a b) -> p a b", a=4)), reads=[PB[b1]], writes=[Bx2T])
                yield
                bl = nbank()
                for kc in range(8):
                    mm(pf(bl, 64), x2Tf[:, kc, :], wr[:, kc, :], kc == 0, kc == 7, [Bx2T, Bwr], PB[bl])
                S.op("act", lambda e, bl=bl: e.activation(out=c2, in_=pf(bl, 64), func=AF.Exp, scale=-1.0), reads=[PB[bl]], writes=R)
                yield
                S.op("dve", lambda e: e.tensor_scalar(out=c2, in0=c2, scalar1=1.0, scalar2=None, op0=ALU.add), reads=R, writes=R)
                yield
                S.op("dve", lambda e: e.reciprocal(out=sc, in_=c2), reads=R, writes=R)
                yield
                S.op("dve", lambda e: e.tensor_tensor(out=ch, in0=sc, in1=rbbc, op=ALU.add), reads=R + [Brc], writes=R)
                yield
                S.op("dve", lambda e: e.tensor_reduce(out=g8(0), in_=V3(ch), axis=AX.X, op=ALU.max), reads=R, writes=R)
                yield
                S.op("dve", lambda e: e.tensor_tensor(out=V3(eq), in0=V3(ch), in1=g8(0).unsqueeze(2).broadcast_to([128, 8, 8]), op=ALU.is_equal), reads=R, writes=R)
                yield
                S.op("dve", lambda e: e.scalar_tensor_tensor(out=c2, in0=eq, scalar=-BIG, in1=ch, op0=ALU.mult, op1=ALU.add), reads=R, writes=R)
                yield
                S.op("dve", lambda e: e.tensor_reduce(out=g8(1), in_=V3(c2), axis=AX.X, op=ALU.max), reads=R, writes=R)
                yield
                S.op("dve", lambda e: e.tensor_tensor(out=g8(2), in0=g8(0), in1=g8(1), op=ALU.add), reads=R, writes=R)
                yield
                S.op("dve", lambda e: e.max(out=g8(3), in_=g8(2)), reads=R, writes=R)
                yield
                S.op("dve", lambda e: e.tensor_scalar(out=g8(4), in0=g8(2), scalar1=RT[:, 9, 27:28], scalar2=None, op0=ALU.is_ge), reads=R, writes=R)
                yield
                S.op("dve", lambda e: e.tensor_scalar(out=g8(5), in0=g8(4), scalar1=-1.0, scalar2=BIG, op0=ALU.add, op1=ALU.mult), reads=R, writes=R)
                yield
                S.op("dve", lambda e: e.tensor_tensor(out=V3(mc), in0=V3(ch), in1=g8(5).unsqueeze(2).broadcast_to([128, 8, 8]), op=ALU.add), reads=R, writes=R)
                yield
                S.op("dve", lambda e: e.max(out=g8(6), in_=mc), reads=R, writes=R)
                yield
                S.op("dve", lambda e: e.tensor_scalar(out=eq, in0=mc, scalar1=RT[:, 9, 55:56], scalar2=None, op0=ALU.is_ge), reads=R, writes=R)
                yield
                S.op("dve", lambda e: e.tensor_tensor(out=w_, in0=sc, in1=eq, op=ALU.mult), reads=R, writes=R)
                yield
                S.op("dve", lambda e: e.tensor_reduce(out=ws1, in_=w_, axis=AX.X, op=ALU.add), reads=R, writes=R)
                yield
                S.op("dve", lambda e: e.reciprocal(out=rs1, in_=ws1), reads=R, writes=R)
                yield
                S.op("dve", lambda e, gwv=gwv: e.tensor_scalar(out=gwv, in0=w_, scalar1=rs1, scalar2=2.5, op0=ALU.mult, op1=ALU.mult), reads=R, writes=[bxr])
                yield
                S.op("dve", lambda e, i=i: e.tensor_copy(out=selall[:, i, :], in_=eq), reads=R, writes=[Bsel[i]])
                yield
                return
            bp = nbank()
            for j in range(i + 1):
                mm(pf(bp, 64), trib[:] if j == i else onesb[:], selall[:, j, :], j == 0, j == i, [Bconst, Bsel[j]], PB[bp])
            S.op("dve", lambda e, bp=bp: e.scalar_tensor_tensor(out=selc, in0=pf(bp, 64), scalar=float(C_CAP), in1=eq, op0=ALU.is_lt, op1=ALU.mult), reads=R + [PB[bp]], writes=R)
            yield
            S.op("dve", lambda e, bp=bp: e.tensor_tensor(out=t1, in0=pf(bp, 64), in1=ecol, op=ALU.add), reads=R + [PB[bp], Brc], writes=R)
            yield
            S.op("dve", lambda e: e.scalar_tensor_tensor(out=key, in0=t1, scalar=1.0, in1=selc, op0=ALU.add, op1=ALU.mult), reads=R, writes=R)
            yield
            S.op("dve", lambda e: e.tensor_scalar(out=key, in0=key, scalar1=-1.0, scalar2=None, op0=ALU.add), reads=R, writes=R)
            yield
            S.op("dve", lambda e: e.max(out=g8(7), in_=key), reads=R, writes=R)
            yield
            S.op("dve", lambda e: e.tensor_scalar(out=g8(6), in0=g8(7), scalar1=0.0, scalar2=float(NSLOT + 1), op0=ALU.is_lt, op1=ALU.mult), reads=R, writes=R)
            yield
            S.op("dve", lambda e: e.tensor_tensor(out=g8(7), in0=g8(7), in1=g8(6), op=ALU.add), reads=R, writes=R)
            yield
            S.op("dve", lambda e, i=i: e.tensor_copy(out=slots[:, i, :], in_=g8(7)), reads=R, writes=[Bslot[i]])
            yield
            S.op("act", lambda e, xr=xr, i=i: e.copy(out=xr[:, 0:1024].rearrange("q (k p) -> q p k", k=8), in_=resid[:, i, :].rearrange("q (p k) -> q p k", k=8)), reads=[BR[i]], writes=[bxr])
            yield
            for k in range(8):
                S.dma("pool", lambda e, xr=xr, i=i, k=k: e.indirect_dma_start(
                    out=xg_d, out_offset=bass.IndirectOffsetOnAxis(ap=slots[:, i, k:k + 1], axis=0), in_=xr, in_offset=None), reads=[bxr, Bslot[i], Bxg], writes=[Buf()])
        qTh = [A(SC2 + 8192 + j * 2048, BF16, 2, 512) for j in range(2)]; BqTh = [Buf("qTh0"), Buf("qTh1")]
        oTg = A(SC2 + 12288, BF16, 8, 512); BoT = Buf("oTg")
        Exs = [A(SC2 + 20480 + i * 1024, BF16, 512) for i in range(4)]; BExs = [Buf("Ex%d" % i) for i in range(4)]
        rdxs = [A(SC2 + 24576 + i * 2048, F32, 512) for i in range(2)]; Brdxs = [Buf("rdx0"), Buf("rdx1")]
        for tg in range(4):
            def qproj(h, tg=tg):
                qT_, bqT_ = qTh[h % 2], BqTh[h % 2]
                for j in range(2):
                    fc = 2 * h + j
                    b = nbank()
                    for kc in range(8):
                        mm(pf(b), wq[:, kc, fc * 128:(fc + 1) * 128], xs_[:, kc, tg * 512:(tg + 1) * 512], kc == 0, kc == 7, [Bwq, BX[tg]], PB[b])
                    S.op("act", lambda e, b=b, j=j, qT_=qT_: e.copy(out=qT_[:, j, :], in_=pf(b)), reads=[PB[b]], writes=[bqT_])

            def logits(h):
                qT_, bqT_ = qTh[h % 2], BqTh[h % 2]
                Ex = Exs[(h % 2) * 2:(h % 2) * 2 + 2]; BEx = BExs[(h % 2) * 2:(h % 2) * 2 + 2]
                for mt in range(2):
                    b = nbank()
                    for j in range(2):
                        mm(pf(b), KmT[:, 2 * h + j, mt * 128:(mt + 1) * 128], qT_[:, j, :], j == 0, j == 1, [BKm, bqT_], PB[b])
                    S.op("act", lambda e, b=b, mt=mt, Ex=Ex: e.activation(out=Ex[mt], in_=pf(b), func=AF.Exp, scale=1.0 / 16.0), reads=[PB[b]], writes=[BEx[mt]])

            def denpv(h):
                Ex = Exs[(h % 2) * 2:(h % 2) * 2 + 2]; BEx = BExs[(h % 2) * 2:(h % 2) * 2 + 2]
                rdx, Brdx = rdxs[h % 2], Brdxs[h % 2]
                bd = nbank()
                for mt in range(2):
                    mm(pf(bd), onesb[:], Ex[mt], mt == 0, mt == 1, [Bconst, BEx[mt]], PB[bd])
                S.op("act", lambda e, bd=bd, rdx=rdx: e.activation(out=rdx, in_=pf(bd), func=AF.Ln), reads=[PB[bd]], writes=[Brdx])
                S.op("act", lambda e, rdx=rdx: e.activation(out=rdx, in_=rdx, func=AF.Exp, scale=-1.0), reads=[Brdx], writes=[Brdx])
                for j in range(2):
                    b = nbank()
                    for mt in range(2):
                        mm(pf(b), Vm[:, mt, (2 * h + j) * 128:(2 * h + j + 1) * 128], Ex[mt], mt == 0, mt == 1, [BVm, BEx[mt]], PB[b])
                    S.op("dve", lambda e, b=b, h=h, j=j, rdx=rdx: e.tensor_tensor(out=oTg[:, 2 * h + j, :], in0=pf(b), in1=rdx, op=ALU.mult),
                         reads=[PB[b], Brdx], writes=[BoT])
            qproj(0)
            flush_pend()
            for h in range(4):
                logits(h)
                if h + 1 < 4:
                    qproj(h + 1)
                denpv(h)
            for p0 in (0, 2):
                pair = (tg * 4 + p0, tg * 4 + p0 + 1)
                for i in pair:
                    il = i - tg * 4
                    for h in range(2):
                        b = nbank()
                        for kc in range(8):
                            mm(pf(b), oTg[:, kc, il * 128:(il + 1) * 128], wo[:, kc, h * 512:(h + 1) * 512], kc == 0, kc == 7, [Bwo, BoT], PB[b])
                        S.op("dve", lambda e, b=b, h=h, i=i: e.scalar_tensor_tensor(out=resid[:, i, h * 512:(h + 1) * 512], in0=resid[:, i, h * 512:(h + 1) * 512],
                                                                                   scalar=ALPHA, in1=pf(b), op0=ALU.mult, op1=ALU.add),
                             reads=[BR[i], PB[b]], writes=[BR[i]])
                flush_pend()
                lockstep([ln_tile(i, resid[:, i, :], BR[i], transpose_to=True, defer=pend, gain_eng="dve", src=resid[:, i, :], srcbuf=BR[i]) for i in pair])
                if stage > 2:
                    pk = pair[0] // 2
                    if pk >= 1:
                        lockstep([c1_tile(i, "A") for i in (2 * pk - 2, 2 * pk - 1)])
                        lockstep([c1_tile(i, "B") for i in (2 * pk - 2, 2 * pk - 1)])
        flush_pend()
        if stage > 2:
            lockstep([c1_tile(i, "A") for i in (14, 15)])
            lockstep([c1_tile(i, "B") for i in (14, 15)])
        S.barrier(skip_q=("pool",) if stage > 2 else ())
        if stage <= 2:
            for i in range(16):
                spdma(out_d[i * 128:(i + 1) * 128, :], resid[:, i, :], [Buf()], reads=[BR[i]])
            S.finish(); S.flush()
            return nc

        wsg = A(SC2, BF16, 8, 256); wsu = A(SC2 + 4096, BF16, 8, 256); wsd = A(SC2 + 8192, BF16, 2, 1024); Bws = Buf("ws")
        wstg = [A(SC2 + 32768, F32, 2048), A(SC2 + 40960, F32, 2048)]; Bwstg = [Buf("wstg0"), Buf("wstg1")]
        spdma(wstg[0].rearrange("p (a b) -> p a b", a=8), sg_d.rearrange("(kc p) f -> p kc f", p=128), [Bwstg[0]])
        spdma(wstg[1].rearrange("p (a b) -> p a b", a=8), su_d.rearrange("(kc p) f -> p kc f", p=128), [Bwstg[1]])
        S.op("dve", lambda e: e.tensor_copy(out=wsg.rearrange("p a b -> p (a b)"), in_=wstg[0]), reads=[Bwstg[0]], writes=[Bws])
        S.op("act", lambda e: e.copy(out=wsu.rearrange("p a b -> p (a b)"), in_=wstg[1]), reads=[Bwstg[1]], writes=[Bws])
        spdma(wstg[0].rearrange("p (a b) -> p a b", a=2), sd_d.rearrange("(fc p) d -> p fc d", p=128), [Bwstg[0]])
        S.op("dve", lambda e: e.tensor_copy(out=wsd.rearrange("p a b -> p (a b)"), in_=wstg[0]), reads=[Bwstg[0]], writes=[Bws])
        hsh = A(SC2 + 12288, BF16, 2, 2048); Bhsh = [Buf("hsh%d" % g) for g in range(4)]
        wbase = [0, 12288]
        wgt = [(A(o, BF16, 8, 256), A(o + 4096, BF16, 8, 256), A(o + 8192, BF16, 2, 1024)) for o in wbase]
        Bwgt = [Buf("wgt0"), Buf("wgt1")]
        pstg = A(SC2 + 65536, F32, 2048); Bpstg = Buf("pstg")
        pre_items = [(e_, m) for e_ in range(2) for m in range(3)]
        pre_state = {"k": 0}

        def pre_step():
            k = pre_state["k"]
            pre_state["k"] += 1
            if 1 <= k <= len(pre_items):
                e_, m = pre_items[k - 1]
                wv = wgt[e_][m].rearrange("p a b -> p (a b)")
                if k % 2 == 0:
                    S.op("dve", lambda e, wv=wv: e.tensor_copy(out=wv, in_=pstg), reads=[Bpstg], writes=[Bwgt[e_]])
                else:
                    S.op("act", lambda e, wv=wv: e.copy(out=wv, in_=pstg), reads=[Bpstg], writes=[Bwgt[e_]])
            if k < len(pre_items):
                e_, m = pre_items[k]
                src = (eg_d[e_].rearrange("(p kc) f -> p (kc f)", kc=8), eu_d[e_].rearrange("(p kc) f -> p (kc f)", kc=8),
                       ed_d[e_].rearrange("(fc p) d -> p fc d", p=128))[m]
                dst = pstg if m < 2 else pstg.rearrange("p (a b) -> p a b", a=2)
                spdma(dst, src, [Bpstg])
        silt = [A(SC2 + 30720 + i * 2048, F32, 512) for i in range(1)]; Bsil = [Buf("sil0")]
        for tg in range(4):
            for f in range(2):
                pre_step()
                bg_, bu_ = nbank(), nbank()
                for kc in range(8):
                    mm(pf(bg_), wsg[:, kc, f * 128:(f + 1) * 128], xs_[:, kc, tg * 512:(tg + 1) * 512], kc == 0, kc == 7, [Bws, BX[tg]], PB[bg_])
                for kc in range(8):
                    mm(pf(bu_), wsu[:, kc, f * 128:(f + 1) * 128], xs_[:, kc, tg * 512:(tg + 1) * 512], kc == 0, kc == 7, [Bws, BX[tg]], PB[bu_])
                S.op("act", lambda e, bg_=bg_: e.activation(out=silt[0], in_=pf(bg_), func=AF.Silu), reads=[PB[bg_]], writes=[Bsil[0]])
                S.op("dve", lambda e, bu_=bu_, f=f, tg=tg: e.tensor_tensor(out=hsh[:, f, tg * 512:(tg + 1) * 512], in0=pf(bu_), in1=silt[0], op=ALU.mult),
                     reads=[PB[bu_], Bsil[0]], writes=[Bhsh[tg]])
        def shared_down(i):
            for h in range(2):
                b = nbank()
                for f in range(2):
                    mm(pf(b), hsh[:, f, i * 128:(i + 1) * 128], wsd[:, f, h * 512:(h + 1) * 512], f == 0, f == 1, [Bws, Bhsh[i // 4]], PB[b])
                S.op("dve", lambda e, b=b, h=h, i=i: e.scalar_tensor_tensor(out=resid[:, i, h * 512:(h + 1) * 512], in0=resid[:, i, h * 512:(h + 1) * 512],
                                                                           scalar=ALPHA, in1=pf(b), op0=ALU.mult, op1=ALU.add),
                     reads=[BR[i], PB[b]], writes=[BR[i]])
        for i in range(16):
            shared_down(i)
        while pre_state["k"] <= len(pre_items):
            pre_step()
        S.barrier()

        stg_off = [SC2 + 32768 + j * 8192 for j in range(5)] + [98304 + 18432]
        stg = [[A(stg_off[ss * 3 + m], F32, 2048) for m in range(3)] for ss in range(2)]
        Bstg = [[Buf("stg%d_%d" % (ss, m)) for m in range(3)] for ss in range(2)]
        hT = [A(24576 + i * 2048, BF16, 2, C_CAP) for i in range(2)]; BhT = [Buf("hT0"), Buf("hT1")]
        sile = [A(28672 + i * 2048, F32, C_CAP) for i in range(2)]; Bsile = [Buf("sile0"), Buf("sile1")]
        xsl = [A(98304 + i * 9216, BF16, NST, 1152) for i in range(2)]; Bxsl = [Buf("xsl%d" % i) for i in range(2)]
        xgT = [A(SC2 + 16384 + i * 8192, BF16, 8, C_CAP) for i in range(2)]; BxgT = [[Buf("xgT%d_%d" % (i, j)) for j in range(2 * NST)] for i in range(2)]
        yE = [A(SC2 + i * 8192, BF16, NST, 1024) for i in range(2)]; ByE = [[Buf("yE%d_%d" % (i, j)) for j in range(2 * NST)] for i in range(2)]
        Bye_d = Buf("ye_d")

        def load_dma(e_):
            srcs = (eg_d[e_].rearrange("(p kc) f -> p (kc f)", kc=8), eu_d[e_].rearrange("(p kc) f -> p (kc f)", kc=8),
                    ed_d[e_].rearrange("(fc p) d -> p fc d", p=128))
            for m in (1, 2, 0):
                sb = stg[e_ % 2][m]
                dst = sb if m < 2 else sb.rearrange("p (a b) -> p a b", a=2)
                spdma(dst, srcs[m], [Bstg[e_ % 2][m]])

        def load_x(e_):
            S.dma("act", lambda e, e_=e_: e.dma_start(out=xsl[e_ % 2], in_=xg_d[e_ * C_CAP:(e_ + 1) * C_CAP, :].rearrange("(s p) f -> p s f", p=128)),
                  reads=[Bxg], writes=[Bxsl[e_ % 2]])

        def load_cast(e_):
            for m, eng in ((1, "dve"), (2, "act"), (0, "pool")):
                wv = wgt[e_ % 2][m].rearrange("p a b -> p (a b)")
                sb = stg[e_ % 2][m]
                if eng == "act":
                    S.op("act", lambda e, wv=wv, sb=sb: e.copy(out=wv, in_=sb), reads=[Bstg[e_ % 2][m]], writes=[Bwgt[e_ % 2]])
                else:
                    S.op(eng, lambda e, wv=wv, sb=sb: e.tensor_copy(out=wv, in_=sb), reads=[Bstg[e_ % 2][m]], writes=[Bwgt[e_ % 2]])

        NEXP = 64
        load_x(0); load_x(1); load_dma(2); load_dma(3)
        for e_ in range(NEXP):
            pp = e_ % 2
            wg_, wu_, wd_ = wgt[e_ % 2]
            Bw_ = Bwgt[e_ % 2]
            for s in range(NST):
                for hb in range(2):
                    b = nbank()
                    for j in range(4):
                        kc = hb * 4 + j
                        mm(PS[:, b, j * 128:(j + 1) * 128], xsl[e_ % 2][:, s, kc * 128:(kc + 1) * 128], identb[:], True, True, [Bxsl[e_ % 2], Bconst], PB[b])
                    if (s * 2 + hb) % 2 == 0:
                        S.op("dve", lambda e, b=b, s=s, hb=hb, pp=pp: e.tensor_copy(out=xgT[pp][:, hb * 4:hb * 4 + 4, s * 128:(s + 1) * 128], in_=pf(b).rearrange("p (a b) -> p a b", a=4)),
                             reads=[PB[b]], writes=[BxgT[pp][s * 2 + hb]])
                    else:
                        S.op("act", lambda e, b=b, s=s, hb=hb, pp=pp: e.copy(out=xgT[pp][:, hb * 4:hb * 4 + 4, s * 128:(s + 1) * 128], in_=pf(b).rearrange("p (a b) -> p a b", a=4)),
                             reads=[PB[b]], writes=[BxgT[pp][s * 2 + hb]])
            for f in range(2):
                bg_, bu_ = nbank(), nbank()
                for kc in range(8):
                    mm(pf(bg_, C_CAP), wg_[:, kc, f * 128:(f + 1) * 128], xgT[pp][:, kc, :], kc == 0, kc == 7, [Bw_] + BxgT[pp], PB[bg_])
                for kc in range(8):
                    mm(pf(bu_, C_CAP), wu_[:, kc, f * 128:(f + 1) * 128], xgT[pp][:, kc, :], kc == 0, kc == 7, [Bw_] + BxgT[pp], PB[bu_])
                S.op("act", lambda e, bg_=bg_, f=f: e.activation(out=sile[f], in_=pf(bg_, C_CAP), func=AF.Silu), reads=[PB[bg_]], writes=[Bsile[f]])
                S.op("dve", lambda e, bu_=bu_, f=f, pp=pp: e.tensor_tensor(out=hT[pp][:, f, :], in0=pf(bu_, C_CAP), in1=sile[f], op=ALU.mult),
                     reads=[PB[bu_], Bsile[f]], writes=[BhT[pp]])
            for s in range(NST):
                gws = xsl[e_ % 2][:, s, 1024 + 2 * e_:1024 + 2 * e_ + 2].bitcast(F32)
                for h in range(2):
                    b = nbank()
                    for f in range(2):
                        mm(pf(b), hT[pp][:, f, s * 128:(s + 1) * 128], wd_[:, f, h * 512:(h + 1) * 512], f == 0, f == 1, [Bw_, BhT[pp]], PB[b])
                    if h == 0:
                        S.op("act", lambda e, b=b, s=s, h=h, pp=pp, gws=gws: e.activation(out=yE[pp][:, s, h * 512:(h + 1) * 512], in_=pf(b), func=AF.Identity, scale=gws),
                             reads=[PB[b], Bxsl[e_ % 2]], writes=[ByE[pp][s * 2 + h]])
                    else:
                        S.op("dve", lambda e, b=b, s=s, h=h, pp=pp, gws=gws: e.tensor_scalar(out=yE[pp][:, s, h * 512:(h + 1) * 512], in0=pf(b), scalar1=gws, scalar2=None, op0=ALU.mult),
                             reads=[PB[b], Bxsl[e_ % 2]], writes=[ByE[pp][s * 2 + h]])
            S.dma("act", lambda e, e_=e_, pp=pp: e.dma_start(out=ye_d[e_ * C_CAP:(e_ + 1) * C_CAP, :].rearrange("(s p) d -> p s d", p=128), in_=yE[pp]),
                  reads=ByE[pp], writes=[Bye_d])
            if e_ + 2 < NEXP:
                load_x(e_ + 2)
                load_cast(e_ + 2)
            if e_ + 4 < NEXP:
                load_dma(e_ + 4)
        S.barrier()

        gb = [[A(98304 + pp * 16384 + k * 2048, BF16, 1024) for k in range(8)] for pp in range(2)]
        Bgb = [[Buf("gb%d_%d" % (pp, k)) for k in range(8)] for pp in range(2)]
        sm = [A(SC2 + j * 4096, F32, 1024) for j in range(4)]; Bsm2 = [Buf("sm%d" % j) for j in range(4)]
        otiles = [A(SC2 + 16384 + j * 4096, F32, 1024) for j in range(2)]; Bots = [Buf("otile0"), Buf("otile1")]
        load_ln(2)
        spdma(ye_d[NSLOT:NSLOT + 1, :], ZT[0:1, 0:1024], [Bye_d], reads=[Bzt])
        Bout = Buf("out")
        lnst["nb"] = 4

        def c3_pre(i):
            pp = i % 2
            for k in range(8):
                S.dma("pool", lambda e, pp=pp, k=k, i=i: e.indirect_dma_start(
                    out=gb[pp][k], out_offset=None, in_=ye_d, in_offset=bass.IndirectOffsetOnAxis(ap=slots[:, i, k:k + 1], axis=0)), reads=[Bye_d, Bslot[i]], writes=[Bgb[pp][k]])
            for h in range(2):
                b = nbank()
                for k in range(8):
                    mm(pf(b), identb[:], gb[pp][k][:, h * 512:(h + 1) * 512], k == 0, k == 7, [Bconst, Bgb[pp][k]], PB[b])
                S.op("dve", lambda e, b=b, h=h, i=i: e.tensor_tensor(out=rbufs[i % 4][:, h * 512:(h + 1) * 512], in0=resid[:, i, h * 512:(h + 1) * 512], in1=pf(b), op=ALU.add),
                     reads=[BR[i], PB[b]], writes=[Brbs[i % 4]])

        for i0_ in range(0, 16, 2):
            c3_pre(i0_); c3_pre(i0_ + 1)
            lockstep([ln_tile(i, otiles[i % 2], Bots[i % 2], transpose_to=None, gain_eng="dve") for i in (i0_, i0_ + 1)])
            for i in (i0_, i0_ + 1):
                spdma(out_d[i * 128:(i + 1) * 128, :], otiles[i % 2], [Buf()], reads=[Bots[i % 2]])
        S.finish()
        S.flush()
    return nc


def _bucket_table():
    d = np.arange(128)
    n = np.maximum(d, 0)
    exact = 16
    large = exact + (np.log(np.maximum(n, 1).astype(np.float32) / exact) / np.float32(np.log(128 / exact)) * (32 - exact)).astype(np.int32)
    large = np.minimum(large, 31)
    return np.where(n < exact, n, large)


_NC_CACHE = {}


def kernel(x, mem, w_in, b_in, conv_w, conv_b, conv_ln_g, conv_ln_b, attn_sinks, rel_bias,
           w_out, b_out, ln1_g, ln1_b, xq_w, xkv_w, xo_w, ln2_g, ln2_b, router_w, router_b,
           exp_gate, exp_up, exp_down, sh_gate, sh_up, sh_down, ln3_g, ln3_b, _stage=99):
    f32 = np.float32
    x = np.asarray(x, f32); mem = np.asarray(mem, f32)
    w_in = np.asarray(w_in, f32)[0]; b_in = np.asarray(b_in, f32)[0]
    qcols = []
    for c in range(4):
        qcols += list(range(1024 + c * 64, 1024 + (c + 1) * 64)) + list(range(1024 + (4 + c) * 64, 1024 + (5 + c) * 64))
    kcols = list(range(1536, 1664))
    v0 = list(range(1664, 1728)); v1 = list(range(1728, 1792))
    cols = list(range(0, 1024)) + qcols + kcols + v0 + v0 + v1 + v1
    win = np.ascontiguousarray(w_in[:, cols])
    bperm = b_in[cols]
    bcols = np.zeros((128, 13), f32)
    for fc in range(4):
        bcols[:, fc] = bperm[fc * 128:(fc + 1) * 128]
        bcols[:, 4 + fc] = bperm[512 + fc * 128:512 + (fc + 1) * 128]
        bcols[:, 8 + fc] = bperm[1024 + fc * 128:1024 + (fc + 1) * 128]
    bcols[:, 12] = bperm[1536:1664]
    bv = np.ascontiguousarray(bperm[1664:1920][None, :])
    cwm = np.asarray(conv_w, f32)[0]
    cw = np.ascontiguousarray(cwm.T.reshape(4, 128, 31).transpose(1, 0, 2).reshape(128, 124))
    cvec = np.zeros((128, 12), f32)
    for j, v in enumerate((conv_b, conv_ln_g, conv_ln_b)):
        cvec[:, 4 * j:4 * j + 4] = np.asarray(v, f32)[0].reshape(4, 128).T
    lnp = np.ascontiguousarray(np.stack([np.asarray(v, f32)[0] for v in (ln1_g, ln1_b, ln2_g, ln2_b, ln3_g, ln3_b)]))
    bk = _bucket_table()
    ohd = np.zeros((32, 128), f32); ohd[bk, np.arange(128)] = 1.0
    ident = np.eye(128, dtype=f32); Jm = np.ascontiguousarray(ident[::-1])
    tri = np.triu(np.ones((128, 128), f32), 1)
    ecol = (np.arange(64, dtype=f32) * C_CAP)[None, :]
    common = dict(
        win=win, bcols=bcols, bv=bv, cw=cw, cvec=cvec, sinks=np.asarray(attn_sinks, f32).reshape(1, 8),
        relb=np.ascontiguousarray(np.asarray(rel_bias, f32)), wout=np.ascontiguousarray(np.asarray(w_out, f32)[0]),
        bout=np.asarray(b_out, f32).reshape(1, 1024), lnp=lnp,
        wq=np.ascontiguousarray(np.asarray(xq_w, f32)[0]), wkv=np.ascontiguousarray(np.asarray(xkv_w, f32)[0]),
        wo=np.ascontiguousarray(np.asarray(xo_w, f32)[0]), wr=np.ascontiguousarray(np.asarray(router_w, f32)[0]),
        rb=np.asarray(router_b, f32).reshape(1, 64),
        eg=np.ascontiguousarray(np.asarray(exp_gate, f32)[0]), eu=np.ascontiguousarray(np.asarray(exp_up, f32)[0]),
        ed=np.ascontiguousarray(np.asarray(exp_down, f32)[0]),
        sg=np.ascontiguousarray(np.asarray(sh_gate, f32)[0]), su=np.ascontiguousarray(np.asarray(sh_up, f32)[0]),
        sd=np.ascontiguousarray(np.asarray(sh_down, f32)[0]),
        ident=ident, Jm=Jm, ohd=ohd, tri=tri, ecol=ecol)
    in_maps = []
    for c in range(8):
        b, half = c // 2, c % 2
        t0 = half * 2048
        xt = np.zeros((1024, 2176), f32)
        xt[:, 128:] = x[b, t0:t0 + 2048].T
        if half == 1:
            xt[:, :128] = x[b, t0 - 128:t0].T
        m = dict(common)
        xtl = np.zeros((128, 8 * 2176), f32)
        for c0_, n_ in [(0, 128)] + [(128 + g_ * 512, 512) for g_ in range(4)]:
            xtl[:, 8 * c0_:8 * (c0_ + n_)] = xt[:, c0_:c0_ + n_].reshape(8, 128, n_).transpose(1, 0, 2).reshape(128, 8 * n_)
        xt = xtl
        m.update(xT=xt, xtok=np.ascontiguousarray(x[b, t0:t0 + 2048]), memT=np.ascontiguousarray(mem[b].T),
                 hv=np.full((128, 1), float(half), f32))
        in_maps.append(m)
    if _stage not in _NC_CACHE:
        _NC_CACHE[_stage] = build_program(_stage)
    nc = _NC_CACHE[_stage]
    res = run_bass_kernel_spmd(nc, in_maps, core_ids=list(range(8)))
    out = np.zeros((4, 4096, 1024), f32)
    for c in range(8):
        b, half = c // 2, c % 2
        out[b, half * 2048:(half + 1) * 2048] = res.results[c]["out"]
    return out
```

```python
import numpy as np
import contextlib
import bisect
import concourse.bass as bass
import concourse.mybir as mybir
from concourse.bass_utils import run_bass_kernel_spmd

F32 = mybir.dt.float32
BF16 = mybir.dt.bfloat16
I32 = mybir.dt.int32
U32 = mybir.dt.uint32
AF = mybir.ActivationFunctionType
ALU = mybir.AluOpType
AX = mybir.AxisListType


class Buf:
    __slots__ = ("w", "r", "name")

    def __init__(self, name=""):
        self.w = {}
        self.r = {}
        self.name = name


class Sched:
    ENG = ("pe", "act", "dve", "pool", "sp")

    def __init__(self, nc, stack, nlanes=32):
        self.nc = nc
        self.sem = {e: stack.enter_context(nc.semaphore("s_" + e)) for e in ("pe", "act", "dve", "pool")}
        self.nl = nlanes
        self.nbg = 64
        self.bg_next = 0
        self.lanes = [stack.enter_context(nc.semaphore("lane%d" % i)) for i in range(nlanes + self.nbg)]
        self.lane_val = [0] * (nlanes + self.nbg)
        self.lane_q = [None] * (nlanes + self.nbg)
        self.lane_rr = 0
        self.sw_rr = 0
        self.hw_rr = 0
        self.q = {e: [] for e in self.ENG}
        self.inc_pos = {e: [] for e in self.ENG}
        self.seen = {e: {} for e in self.ENG}
        self.nops = 0

    def _resolve(self, eng, idx):
        ip = self.inc_pos[eng]
        j = bisect.bisect_left(ip, idx)
        if j == len(ip):
            ent = self.q[eng][idx]
            assert ent[0] == "op"
            ent[2] = True
            ip.append(idx)
        return j + 1

    def _wait(self, eng, key, v):
        if key[0] == "e":
            sem = self.sem[key[1]]
            val = self._resolve(key[1], v)
        else:
            sem = self.lanes[key[1]]
            val = v
        if self.seen[eng].get(key, 0) >= val:
            return
        self.seen[eng][key] = val
        self.q[eng].append(["wait", sem, val])

    def _deps(self, eng, reads, writes):
        deps = {}
        for b in reads:
            for k, v in b.w.items():
                if deps.get(k, -1) < v:
                    deps[k] = v
        for b in writes:
            for d in (b.w, b.r):
                for k, v in d.items():
                    if deps.get(k, -1) < v:
                        deps[k] = v
        for k, v in deps.items():
            if eng == "pe" and k == ("e", "pe"):
                continue
            self._wait(eng, k, v)

    def _mark(self, key, val, reads, writes):
        for b in reads:
            if b.r.get(key, -1) < val:
                b.r[key] = val
        for b in writes:
            b.w = {key: val}
            b.r = {}

    def op(self, eng, fn, reads=(), writes=()):
        self._deps(eng, reads, writes)
        idx = len(self.q[eng])
        self.q[eng].append(["op", fn, False])
        self._mark(("e", eng), idx, reads, writes)
        self.nops += 1

    def dma(self, qeng, fn, reads=(), writes=(), bg=False):
        if bg:
            lane = self.nl + self.bg_next
            self.bg_next += 1
            assert self.bg_next <= self.nbg
        else:
            half = self.nl // 2
            if qeng == "pool":
                lane = self.sw_rr
                self.sw_rr = (lane + 1) % half
            else:
                lane = half + self.hw_rr
                self.hw_rr = (self.hw_rr + 1) % half
        if self.lane_val[lane] > 0:
            self._wait(qeng, ("l", lane), self.lane_val[lane])
        self._deps(qeng, reads, writes)
        self.lane_val[lane] += 16
        self.lane_q[lane] = qeng
        self.q[qeng].append(["dma", fn, lane])
        self._mark(("l", lane), self.lane_val[lane], reads, writes)
        self.nops += 1

    def barrier(self, final=False, skip_q=()):
        last = {}
        for e in ("pe", "act", "dve", "pool"):
            for i in range(len(self.q[e]) - 1, -1, -1):
                if self.q[e][i][0] == "op":
                    last[e] = i
                    break
        for e in self.ENG:
            for f, i in last.items():
                if f != e:
                    self._wait(e, ("e", f), i)
            for l, v in enumerate(self.lane_val):
                if v > 0 and (final or l < self.nl) and self.lane_q[l] not in skip_q:
                    self._wait(e, ("l", l), v)

    def finish(self):
        self.barrier(final=True)

    def flush(self):
        sched = self
        with self.nc.Block() as block:
            for e, deco in (("pe", block.tensor), ("act", block.scalar), ("dve", block.vector),
                            ("pool", block.gpsimd), ("sp", block.sync)):
                entries = self.q[e]

                def body(engine, entries=entries, e=e):
                    for ent in entries:
                        if ent[0] == "wait":
                            engine.wait_ge(ent[1], ent[2])
                        elif ent[0] == "op":
                            ins = ent[1](engine)
                            if ent[2]:
                                ins.then_inc(sched.sem[e], 1)
                        else:
                            ins = ent[1](engine)
                            ins.then_inc(sched.lanes[ent[2]], 16)
                deco(body)


C_CAP = 512
NST = C_CAP // 128
NSLOT = 64 * C_CAP
ALPHA = 2.0 ** 0.25
LN_EPS = 1e-5
ARENA_BYTES = 200 * 1024


def build_program(stage=99):
    nc = bass.Bass("TRN2", target_bir_lowering=False)

    def din(name, shape, dt=F32):
        return nc.dram_tensor(name, list(shape), dt, kind="ExternalInput").ap()

    xT_d = din("xT", [128, 8 * 2176])
    xtok_d = din("xtok", [2048, 1024]); memT_d = din("memT", [1024, 256])
    hv_d = din("hv", [128, 1])
    win_d = din("win", [1024, 1920]); bcols_d = din("bcols", [128, 13]); bv_d = din("bv", [1, 256])
    cw_d = din("cw", [128, 124]); cvec_d = din("cvec", [128, 12])
    sinks_d = din("sinks", [1, 8]); relb_d = din("relb", [32, 8])
    wout_d = din("wout", [1024, 1024]); bout_d = din("bout", [1, 1024]); lnp_d = din("lnp", [6, 1024])
    wq_d = din("wq", [1024, 1024]); wkv_d = din("wkv", [1024, 2048]); wo_d = din("wo", [1024, 1024])
    wr_d = din("wr", [1024, 64]); rb_d = din("rb", [1, 64])
    eg_d = din("eg", [64, 1024, 256]); eu_d = din("eu", [64, 1024, 256]); ed_d = din("ed", [64, 256, 1024])
    sg_d = din("sg", [1024, 256]); su_d = din("su", [1024, 256]); sd_d = din("sd", [256, 1024])
    ident_d = din("ident", [128, 128]); J_d = din("Jm", [128, 128]); ohd_d = din("ohd", [32, 128])
    tri_d = din("tri", [128, 128]); ecol_d = din("ecol", [1, 64])
    out_d = nc.dram_tensor("out", [2048, 1024], F32, kind="ExternalOutput").ap()
    xg_d = nc.dram_tensor("xg_scr", [NSLOT + 1, 1152], BF16).ap()
    ye_d = nc.dram_tensor("ye_scr", [NSLOT + 1, 1024], BF16).ap()
    rrow_t = nc.dram_tensor("rrow_scr", [8, 384], F32)
    rrow_d = rrow_t.ap()

    with contextlib.ExitStack() as st:
        S = Sched(nc, st)
        ARENA = st.enter_context(nc.sbuf_tensor("arena", [128, ARENA_BYTES // 2], BF16))
        PRM = st.enter_context(nc.sbuf_tensor("prm", [128, 192], F32))
        SMALL = st.enter_context(nc.sbuf_tensor("small", [128, 4, 32], F32))
        SLOTS = st.enter_context(nc.sbuf_tensor("slots", [128, 16, 8], I32))
        ZT = st.enter_context(nc.sbuf_tensor("zt", [128, 1152], BF16))
        identb = st.enter_context(nc.sbuf_tensor("identb", [128, 128], BF16))
        identf = st.enter_context(nc.sbuf_tensor("identf", [128, 128], F32))
        Jf = st.enter_context(nc.sbuf_tensor("Jf", [128, 128], F32))
        trib = st.enter_context(nc.sbuf_tensor("trib", [128, 128], BF16))
        onesb = st.enter_context(nc.sbuf_tensor("onesb", [128, 128], BF16))
        onesdiv = st.enter_context(nc.sbuf_tensor("onesdiv", [128, 128], F32))
        cstage = st.enter_context(nc.sbuf_tensor("cstage", [128, 128], F32))
        PS = st.enter_context(nc.psum_tensor("ps", [128, 8, 512], F32))

        def A(off, dt, *shape):
            n = 1
            for s in shape:
                n *= s
            if dt == BF16:
                ap = ARENA[:, off // 2: off // 2 + n]
            else:
                ap = ARENA[:, off // 2: off // 2 + 2 * n].bitcast(dt)
            if len(shape) == 2:
                ap = ap.rearrange("p (a b) -> p a b", a=shape[0])
            elif len(shape) == 3:
                ap = ap.rearrange("p (a b c) -> p a b c", a=shape[0], b=shape[1])
            return ap

        PB = [Buf("psum%d" % i) for i in range(8)]
        pstate = {"rr": 0}

        def nbank():
            b = pstate["rr"]
            pstate["rr"] = (b + 1) % 8
            return b

        def pf(b, n=512):
            return PS[:, b, 0:n]

        def pbf(b):
            return PS[:, b, :].bitcast(BF16)

        def mm(out, lhsT, rhs, start, stop, reads, wbuf):
            S.op("pe", lambda e: e.matmul(out, lhsT=lhsT, rhs=rhs, start=start, stop=stop), reads=reads, writes=[wbuf])

        def castdma(out, in_, writes, reads=()):
            S.dma("pool", lambda e: e.dma_start(out=out, in_=in_), reads=reads, writes=writes)

        def spdma(out, in_, writes, reads=()):
            S.dma("sp", lambda e: e.dma_start(out=out, in_=in_), reads=reads, writes=writes)

        Bprm = Buf("prm"); Bconst = Buf("const"); Bcst = Buf("cstage")
        spdma(PRM[:, 0:13], bcols_d, [Bprm])
        spdma(PRM[:, 13:14], hv_d, [Bprm])
        spdma(PRM[:, 16:28], cvec_d, [Bprm])
        spdma(PRM[:, 32:156], cw_d, [Bprm])
        spdma(PRM[:, 160:168], sinks_d[0:1, :].broadcast_to([128, 8]), [Bprm])
        S.op("dve", lambda e: e.memset(PRM[:, 14:15], LN_EPS), writes=[Bprm])
        S.op("act", lambda e: e.activation(out=PRM[:, 168:176], in_=PRM[:, 160:168], func=AF.Exp), reads=[Bprm], writes=[Bprm])
        spdma(identf[:], ident_d, [Bconst])
        spdma(Jf[:], J_d, [Bconst])
        S.op("dve", lambda e: e.tensor_copy(out=identb[:], in_=identf[:]), reads=[Bconst], writes=[Bconst])
        spdma(cstage[:], tri_d, [Bcst])
        S.op("dve", lambda e: e.tensor_copy(out=trib[:], in_=cstage[:]), reads=[Bcst], writes=[Bconst])
        S.op("dve", lambda e: e.memset(onesb[:], 1.0), writes=[Bconst])
        S.op("dve", lambda e: e.memset(onesdiv[:], 1.0 / 512.0), writes=[Bconst])
        b_a = lambda fc: PRM[:, fc:fc + 1]
        b_g = lambda fc: PRM[:, 4 + fc:5 + fc]
        b_q = lambda fc: PRM[:, 8 + fc:9 + fc]
        b_k = PRM[:, 12:13]
        hv = PRM[:, 13:14]
        eps = PRM[:, 14:15]
        convb = lambda fc: PRM[:, 16 + fc:17 + fc]
        clng = lambda fc: PRM[:, 20 + fc:21 + fc]
        clnb = lambda fc: PRM[:, 24 + fc:25 + fc]
        cwk = lambda fc, k: PRM[:, 32 + fc * 31 + k:33 + fc * 31 + k]

        mixT = A(0, BF16, 8, 2048); BmixC = [Buf("mixc%d" % g) for g in range(4)]; BmixA = [Buf("mixa%d" % n) for n in range(16)]
        QT = A(32768, BF16, 4, 2048); BQ = [Buf("q%d" % g) for g in range(4)]
        KT = A(49152, BF16, 2176); BK = [Buf("k%d" % g) for g in range(5)]
        Vt = A(53504, BF16, 17, 256); BV = [Buf("v%d" % g) for g in range(5)]
        uT = A(62208, BF16, 4, 2176); BU = [Buf("u%d" % g) for g in range(5)]
        SCR = 79616

        win = A(SCR, BF16, 8, 1920)
        Bwin = [Buf("win%d" % kc) for kc in range(8)]
        winstg = [A(SCR + 86016 + j * 7680, F32, 1920) for j in range(2)]; Bwinstg = [Buf("winstg0"), Buf("winstg1")]
        for kc in range(4):
            castdma(win[:, kc, :], win_d[kc * 128:(kc + 1) * 128, :], [Bwin[kc]])
        for kc in range(4, 8):
            j = kc % 2
            spdma(winstg[j], win_d[kc * 128:(kc + 1) * 128, :], [Bwinstg[j]])
            if j == 0:
                S.op("dve", lambda e, kc=kc, j=j: e.tensor_copy(out=win[:, kc, :], in_=winstg[j]), reads=[Bwinstg[j]], writes=[Bwin[kc]])
            else:
                S.op("act", lambda e, kc=kc, j=j: e.copy(out=win[:, kc, :], in_=winstg[j]), reads=[Bwinstg[j]], writes=[Bwin[kc]])
        xTg = [A(SCR + 30720 + i * 8192, BF16, 8, 512) for i in range(2)]; BxTg = [Buf("xtg0"), Buf("xtg1")]
        sig = [A(SCR + 30720 + 16384 + i * 2048, F32, 512) for i in range(2)]; Bsig = [Buf("sig0"), Buf("sig1")]
        bvbc = A(SCR + 30720 + 16384 + 4096, F32, 256); Bbv = Buf("bv")
        spdma(bvbc, bv_d[0:1, :].broadcast_to([128, 256]), [Bbv])
        groups = [(0, 128)] + [(128 + g * 512, 512) for g in range(4)]
        def load_x(gi):
            c0, n = groups[gi]
            castdma(xTg[gi % 2].rearrange("p a b -> p (a b)")[:, 0:8 * n], xT_d[:, 8 * c0:8 * (c0 + n)], [BxTg[gi % 2]])
        load_x(0); load_x(1)
        diag = A(SCR + 53248, BF16, 124, 128); Bdiag = [[Buf("diag%d_%d" % (fc, k)) for k in range(31)] for fc in range(4)]

        def build_diag(fc):
            for k in range(31):
                eng = ("dve", "act")[k % 2]
                if eng == "act":
                    S.op("act", lambda e, fc=fc, k=k: e.activation(out=diag[:, fc * 31 + k, :], in_=identf[:], func=AF.Identity, scale=cwk(fc, k)),
                         reads=[Bconst, Bprm], writes=[Bdiag[fc][k]])
                else:
                    S.op(eng, lambda e, fc=fc, k=k: e.tensor_scalar(out=diag[:, fc * 31 + k, :], in0=identf[:], scalar1=cwk(fc, k), scalar2=None, op0=ALU.mult),
                         reads=[Bconst, Bprm], writes=[Bdiag[fc][k]])
        for gi, (c0, n) in enumerate(groups):
            xb_, bx = xTg[gi % 2], BxTg[gi % 2]
            if n != 512:
                xb_ = xb_.rearrange("p a b -> p (a b)")[:, 0:8 * n].rearrange("p (a b) -> p a b", a=8)
            if gi >= 2:
                load_x(gi)
            for fc in range(4):
                ba, bg = nbank(), nbank()
                for kc in range(8):
                    mm(pf(ba, n), win[:, kc, fc * 128:(fc + 1) * 128], xb_[:, kc, 0:n], kc == 0, kc == 7, [Bwin[kc], bx], PB[ba])
                for kc in range(8):
                    mm(pf(bg, n), win[:, kc, 512 + fc * 128:512 + (fc + 1) * 128], xb_[:, kc, 0:n], kc == 0, kc == 7, [Bwin[kc], bx], PB[bg])
                sg_, bs = sig[fc % 2], Bsig[fc % 2]
                S.op("act", lambda e, sg_=sg_, bg=bg, fc=fc, n=n: e.activation(out=sg_[:, 0:n], in_=pf(bg, n), func=AF.Sigmoid, bias=b_g(fc)),
                     reads=[PB[bg], Bprm], writes=[bs])
                S.op("dve", lambda e, sg_=sg_, ba=ba, fc=fc, n=n, c0=c0: e.scalar_tensor_tensor(
                    out=uT[:, fc, c0:c0 + n], in0=pf(ba, n), scalar=b_a(fc), in1=sg_[:, 0:n], op0=ALU.add, op1=ALU.mult),
                    reads=[PB[ba], bs, Bprm], writes=[BU[gi]])
                if gi == 0:
                    S.op("dve", lambda e, fc=fc: e.tensor_scalar(out=uT[:, fc, 0:128], in0=uT[:, fc, 0:128], scalar1=hv, scalar2=None, op0=ALU.mult),
                         reads=[BU[0], Bprm], writes=[BU[0]])
            if gi > 0:
                t0 = c0 - 128
                for fc in range(4):
                    bq = nbank()
                    for kc in range(8):
                        mm(pf(bq, n), win[:, kc, 1024 + fc * 128:1024 + (fc + 1) * 128], xb_[:, kc, 0:n], kc == 0, kc == 7, [Bwin[kc], bx], PB[bq])
                    S.op("act", lambda e, bq=bq, fc=fc, t0=t0, n=n: e.activation(out=QT[:, fc, t0:t0 + n], in_=pf(bq, n), func=AF.Identity, bias=b_q(fc)),
                         reads=[PB[bq], Bprm], writes=[BQ[gi - 1]])
            bk_ = nbank()
            for kc in range(8):
                mm(pf(bk_, n), win[:, kc, 1536:1664], xb_[:, kc, 0:n], kc == 0, kc == 7, [Bwin[kc], bx], PB[bk_])
            S.op("act", lambda e, bk_=bk_, c0=c0, n=n: e.activation(out=KT[:, c0:c0 + n], in_=pf(bk_, n), func=AF.Identity, bias=b_k),
                 reads=[PB[bk_], Bprm], writes=[BK[gi]])
            if gi < 4:
                build_diag(gi)
            for j in range(n // 128):
                bvv = nbank()
                for kc in range(8):
                    mm(pf(bvv, 256), xb_[:, kc, j * 128:(j + 1) * 128], win[:, kc, 1664:1920], kc == 0, kc == 7, [Bwin[kc], bx], PB[bvv])
                ti = c0 // 128 + j
                S.op("dve", lambda e, bvv=bvv, ti=ti: e.tensor_tensor(out=Vt[:, ti, :], in0=pf(bvv, 256), in1=bvbc, op=ALU.add),
                     reads=[PB[bvv], Bbv], writes=[BV[gi]])
        S.barrier()

        Bzt = Buf("zt"); Bxg = Buf("xg_d")
        S.op("dve", lambda e: e.memset(ZT[:], 0.0), writes=[Bzt])
        def zero_fill(r):
            S.dma("sp", lambda e, r=r: e.dma_start(out=xg_d[r * 512:(r + 1) * 512, :].rearrange("(p a) f -> p a f", a=4),
                                                   in_=ZT[:].unsqueeze(1).broadcast_to([128, 4, 1152])), reads=[Bzt], writes=[Bxg], bg=True)
        yf = A(SCR, F32, 4, 512); Byf = Buf("yf")
        ysq = A(SCR + 8192, F32, 4, 512); Bysq = Buf("ysq")
        stt = [A(SCR + 16384 + i * 2048, F32, 512) for i in range(5)]; Bst = [Buf("st%d" % i) for i in range(5)]
        ctmp = [A(SCR + 16384 + 10240 + i * 2048, F32, 512) for i in range(4)]; Bct = [Buf("ct%d" % i) for i in range(4)]
        for tg in range(4):
            pbk = []
            for fc in range(4):
                zero_fill(tg * 4 + fc)
                b = nbank(); pbk.append(b)
                for k in range(31):
                    c0 = 128 + tg * 512 - 30 + k
                    mm(pf(b), diag[:, fc * 31 + k, :], uT[:, fc, c0:c0 + 512], k == 0, k == 30, [Bdiag[fc][k]] + BU, PB[b])
                S.op("act", lambda e, b=b, fc=fc: e.activation(out=yf[:, fc, :], in_=pf(b), func=AF.Identity, bias=convb(fc)),
                     reads=[PB[b], Bprm], writes=[Byf])
                S.op("act", lambda e, b=b, fc=fc: e.activation(out=ysq[:, fc, :], in_=pf(b), func=AF.Square, bias=convb(fc)),
                     reads=[PB[b], Bprm], writes=[Bysq])
            bm, bs2 = nbank(), nbank()
            for fc in range(4):
                mm(pf(bm), onesdiv[:], yf[:, fc, :], fc == 0, fc == 3, [Bconst, Byf], PB[bm])
            for fc in range(4):
                mm(pf(bs2), onesdiv[:], ysq[:, fc, :], fc == 0, fc == 3, [Bconst, Bysq], PB[bs2])
            mean, m2, var, sd, rstd = stt
            S.op("act", lambda e, bm=bm: e.copy(out=mean, in_=pf(bm)), reads=[PB[bm]], writes=[Bst[0]])
            S.op("dve", lambda e: e.tensor_tensor(out=m2, in0=mean, in1=mean, op=ALU.mult), reads=[Bst[0]], writes=[Bst[1]])
            S.op("dve", lambda e, bs2=bs2: e.scalar_tensor_tensor(out=var, in0=m2, scalar=-1.0, in1=pf(bs2), op0=ALU.mult, op1=ALU.add),
                 reads=[Bst[1], PB[bs2]], writes=[Bst[2]])
            S.op("act", lambda e: e.activation(out=sd, in_=var, func=AF.Sqrt, bias=eps), reads=[Bst[2], Bprm], writes=[Bst[3]])
            S.op("dve", lambda e: e.reciprocal(out=rstd, in_=sd), reads=[Bst[3]], writes=[Bst[4]])
            for fc in range(4):
                cen, bc = ctmp[fc], Bct[fc]
                S.op("dve", lambda e, cen=cen, fc=fc: e.tensor_tensor(out=cen, in0=yf[:, fc, :], in1=mean, op=ALU.subtract),
                     reads=[Byf, Bst[0]], writes=[bc])
                S.op("dve", lambda e, cen=cen: e.tensor_tensor(out=cen, in0=cen, in1=rstd, op=ALU.mult), reads=[bc, Bst[4]], writes=[bc])
                S.op("act", lambda e, cen=cen, fc=fc, tg=tg: e.activation(out=mixT[:, fc, tg * 512:(tg + 1) * 512], in_=cen, func=AF.Silu,
                                                                         scale=clng(fc), bias=clnb(fc)),
                     reads=[bc, Bprm], writes=[BmixC[tg]])
        S.barrier()

        SC2 = 131072
        wout = A(SC2, BF16, 8, 1024); Bwout = Buf("wout")
        wostg = A(SC2 + 61440, F32, 2048); Bwostg = Buf("wostg")
        for c in range(4):
            spdma(wostg.rearrange("p (a b) -> p a b", a=2), wout_d[c * 256:(c + 1) * 256, :].rearrange("(kc p) f -> p kc f", p=128), [Bwostg])
            S.op("act", lambda e, c=c: e.copy(out=wout[:, 2 * c:2 * c + 2, :], in_=wostg.rearrange("p (a b) -> p a b", a=2)), reads=[Bwostg], writes=[Bwout])
        boutb = A(SC2 + 16384, BF16, 1024); Bbout = Buf("bout")
        castdma(boutb[0:1, :], bout_d, [Bbout])
        bexp = A(SCR, BF16, 3, 8, 128); Bbexp = Buf("bexp")
        tilep = [A(SCR + 6144 + i * 4096, F32, 8, 128) for i in range(2)]; Btp = [Buf("tp0"), Buf("tp1")]
        Eb = [A(SCR + 14336 + i * 1024, BF16, 512) for i in range(8)]; BE = [Buf("E%d" % i) for i in range(8)]
        Em = [A(SCR + 22528 + i * 1024, BF16, 512) for i in range(8)]; BEm = [Buf("Em%d" % i) for i in range(8)]
        rden = [A(SCR + 37888 + i * 2048, F32, 512) for i in range(4)]; Brden = [Buf("rden%d" % i) for i in range(4)]
        esrow = A(SCR + 46080, BF16, 2, 512); Besr = Buf("esrow")
        rrow_s = A(SCR + 34816, F32, 384); Brr = Buf("rrow_s"); Brd = Buf("rrow_d")
        ohd_s = A(SCR + 36352, F32, 128); relb_s = A(SCR + 36864, F32, 8); Boh = Buf("ohd")
        spdma(ohd_s[0:32, :], ohd_d, [Boh]); spdma(relb_s[0:32, :], relb_d, [Boh])
        S.op("dve", lambda e: e.memset(rrow_s[0:8, :], 0.0), writes=[Brr])
        bt = nbank()
        mm(PS[0:8, bt, 0:128], relb_s[0:32, :], ohd_s[0:32, :], True, True, [Boh], PB[bt])
        S.op("act", lambda e: e.activation(out=rrow_s[0:8, 128:256], in_=PS[0:8, bt, 0:128], func=AF.Exp), reads=[PB[bt]], writes=[Brr])
        spdma(rrow_d, rrow_s[0:8, :], [Brd], reads=[Brr])
        for part, off in ((1, 1), (0, 129)):
            tp, btp = tilep[part], Btp[part]
            spdma(tp, bass.AP(rrow_t, off, [[1, 128], [384, 8], [1, 128]]), [btp], reads=[Brd])
            for hh in range(2):
                b = nbank()
                mm(pf(b), Jf[:], tp[:, hh * 4:(hh + 1) * 4, :], True, True, [Bconst, btp], PB[b])
                S.op("dve", lambda e, b=b, part=part, hh=hh: e.tensor_copy(out=bexp[:, part, hh * 4:(hh + 1) * 4, :],
                                                                         in_=pf(b).rearrange("p (a b) -> p a b", a=4)),
                     reads=[PB[b]], writes=[Bbexp])
        S.op("dve", lambda e: e.tensor_scalar(out=bexp[:, 2, :, :], in0=bexp[:, 0, :, :], scalar1=hv, scalar2=None, op0=ALU.mult),
             reads=[Bbexp, Bprm], writes=[Bbexp])
        for kv in range(2):
            S.op("dve", lambda e, kv=kv: e.tensor_copy(out=esrow[0:1, kv, :].rearrange("p (a b) -> p a b", a=4),
                                                      in_=PRM[0:1, 168 + kv * 4:172 + kv * 4].unsqueeze(2).broadcast_to([1, 4, 128])),
                 reads=[Bprm], writes=[Besr])
        def a3_S(n, kv):
                zero_fill(16 + n * 2 + kv)
                r0 = kv * 64
                ii = (n % 2) * 2 + kv
                for part in range(2):
                    b = nbank()
                    kc0 = (n + part) * 128
                    mm(pf(b), KT[r0:r0 + 64, kc0:kc0 + 128], QT[r0:r0 + 64, :, n * 128:(n + 1) * 128], True, True, BK + BQ, PB[b])
                    ei = ii * 2 + part
                    S.op("act", lambda e, b=b, ei=ei: e.activation(out=Eb[ei], in_=pf(b), func=AF.Exp, scale=0.125), reads=[PB[b]], writes=[BE[ei]])
                    bsel = (2 if n == 0 else 0) if part == 0 else 1
                    eng = "dve"
                    S.op(eng, lambda e, ei=ei, bsel=bsel, kv=kv: e.tensor_tensor(
                        out=Em[ei], in0=Eb[ei], in1=bexp[:, bsel, kv * 4:(kv + 1) * 4, :].rearrange("p a b -> p (a b)"), op=ALU.mult),
                        reads=[BE[ei], Bbexp], writes=[BEm[ei]])

        def a3_P(n, kv):
                ii = (n % 2) * 2 + kv
                bo, bd = nbank(), nbank()
                for part in range(2):
                    ei = ii * 2 + part
                    mm(pf(bo), Vt[:, n + part, kv * 128:(kv + 1) * 128], Em[ei], part == 0, part == 1, BV + [BEm[ei]], PB[bo])
                for part in range(2):
                    ei = ii * 2 + part
                    mm(pf(bd), onesb[:], Em[ei], part == 0, False, [Bconst, BEm[ei]], PB[bd])
                mm(pf(bd), onesb[0:1, :], esrow[0:1, kv, :], False, True, [Bconst, Besr], PB[bd])
                S.op("act", lambda e, ii=ii, bd=bd: e.activation(out=rden[ii], in_=pf(bd), func=AF.Ln), reads=[PB[bd]], writes=[Brden[ii]])
                S.op("act", lambda e, ii=ii: e.activation(out=rden[ii], in_=rden[ii], func=AF.Exp, scale=-1.0), reads=[Brden[ii]], writes=[Brden[ii]])
                for i in range(2):
                    p0 = 64 * i
                    S.op("dve", lambda e, bo=bo, ii=ii, kv=kv, i=i, p0=p0, n=n: e.tensor_tensor(
                        out=mixT[p0:p0 + 64, 4 + 2 * kv:6 + 2 * kv, n * 128:(n + 1) * 128],
                        in0=PS[p0:p0 + 64, bo, :].rearrange("p (j i q) -> p j i q", j=2, i=2)[:, :, i, :],
                        in1=rden[ii][p0:p0 + 64, :].rearrange("p (j i q) -> p j i q", j=2, i=2)[:, :, i, :], op=ALU.mult),
                        reads=[PB[bo], Brden[ii]], writes=[BmixA[n]])
        a3_list = [(n, kv) for n in range(16) for kv in range(2)]
        a3_S(*a3_list[0])
        for j_, nk in enumerate(a3_list):
            if j_ + 1 < len(a3_list):
                a3_S(*a3_list[j_ + 1])
            a3_P(*nk)
        S.barrier()

        resid = A(32768, F32, 16, 1024); BR = [Buf("res%d" % i) for i in range(16)]
        xs_ = A(98304, BF16, 8, 2048); BX = [Buf("xs%d" % g) for g in range(4)]
        SC2 = 131072
        lnbc = A(SC2 + 28672, F32, 2, 1024); Blnbc = Buf("lnbc")
        xbbs = [A(SC2 + 36864 + j * 2048, BF16, 1024) for j in range(4)]; Bxbbs = [Buf("xbb%d" % j) for j in range(4)]
        rbufs = [A(SC2 + 45056 + j * 4096, F32, 1024) for j in range(4)]; Brbs = [Buf("rbuf%d" % j) for j in range(4)]
        Bsms = [Buf("small%d" % j) for j in range(4)]

        def lockstep(gens):
            gens = list(gens)
            while gens:
                for g_ in list(gens):
                    try:
                        next(g_)
                    except StopIteration:
                        gens.remove(g_)

        def run(g_):
            for _ in g_:
                pass
        lnst = {"nb": 2}

        def load_ln(idx):
            spdma(lnbc[:, 0, :], lnp_d[2 * idx:2 * idx + 1, :].broadcast_to([128, 1024]), [Blnbc])
            spdma(lnbc[:, 1, :], lnp_d[2 * idx + 1:2 * idx + 2, :].broadcast_to([128, 1024]), [Blnbc])

        def ln_tile(i, dst, dstbuf, transpose_to=None, gain_eng="pool", defer=None, src=None, srcbuf=None):
            nb_ = lnst["nb"]
            rbuf, Brb, xbb, Bxbb, Bsm = rbufs[i % nb_], Brbs[i % nb_], xbbs[i % nb_], Bxbbs[i % nb_], Bsms[i % nb_]
            if src is not None:
                rbuf, Brb = src, srcbuf
            SM = SMALL[:, i % nb_, :]
            stats = SM[:, 0:12]; mv = SM[:, 12:14]; sdv = SM[:, 14:15]; rsv = SM[:, 15:16]; nbv = SM[:, 16:17]
            S.op("dve", lambda e: e.bn_stats(out=SM[:, 0:6], in_=rbuf[:, 0:512]), reads=[Brb], writes=[Bsm])
            yield
            S.op("dve", lambda e: e.bn_stats(out=SM[:, 6:12], in_=rbuf[:, 512:1024]), reads=[Brb], writes=[Bsm])
            yield
            S.op("dve", lambda e: e.bn_aggr(out=mv, in_=stats), reads=[Bsm], writes=[Bsm])
            yield
            S.op("act", lambda e: e.activation(out=sdv, in_=SM[:, 13:14], func=AF.Ln, bias=eps), reads=[Bsm, Bprm], writes=[Bsm])
            yield
            S.op("act", lambda e: e.activation(out=rsv, in_=sdv, func=AF.Exp, scale=-0.5), reads=[Bsm], writes=[Bsm])
            yield
            S.op("dve", lambda e: e.scalar_tensor_tensor(out=nbv, in0=SM[:, 12:13], scalar=-1.0, in1=rsv, op0=ALU.mult, op1=ALU.mult),
                 reads=[Bsm], writes=[Bsm])
            yield
            S.op("act", lambda e: e.activation(out=dst, in_=rbuf, func=AF.Identity, scale=rsv, bias=nbv), reads=[Brb, Bsm], writes=[dstbuf])
            yield
            S.op(gain_eng, lambda e: e.tensor_tensor(out=dst, in0=dst, in1=lnbc[:, 0, :], op=ALU.mult), reads=[dstbuf, Blnbc], writes=[dstbuf])
            yield
            S.op(gain_eng, lambda e: e.tensor_tensor(out=dst, in0=dst, in1=lnbc[:, 1, :], op=ALU.add), reads=[dstbuf, Blnbc], writes=[dstbuf])
            yield
            if transpose_to is not None:
                S.op("act", lambda e: e.copy(out=xbb, in_=dst), reads=[dstbuf], writes=[Bxbb])
                yield

                def tr_part():
                    for hb in range(2):
                        b = nbank()
                        for j in range(4):
                            kc = hb * 4 + j
                            mm(PS[:, b, j * 128:(j + 1) * 128], xbb[:, kc * 128:(kc + 1) * 128], identb[:], True, True, [Bxbb, Bconst], PB[b])
                        if hb == 0:
                            S.op("dve", lambda e, b=b, i=i, hb=hb: e.tensor_copy(out=xs_[:, hb * 4:hb * 4 + 4, i * 128:(i + 1) * 128], in_=pf(b).rearrange("p (a b) -> p a b", a=4)),
                                 reads=[PB[b]], writes=[BX[i // 4]])
                        else:
                            S.op("act", lambda e, b=b, i=i, hb=hb: e.copy(out=xs_[:, hb * 4:hb * 4 + 4, i * 128:(i + 1) * 128], in_=pf(b).rearrange("p (a b) -> p a b", a=4)),
                                 reads=[PB[b]], writes=[BX[i // 4]])
                if defer is None:
                    tr_part()
                else:
                    defer.append(tr_part)

        xt = [A(SC2 + 18432 + i * 4096, F32, 1024) for i in range(2)]; Bxt = [Buf("xt0"), Buf("xt1")]
        load_ln(0)
        pend = []

        def flush_pend(keep=0):
            while len(pend) > keep:
                pend.pop(0)()
        lnst["nb"] = 4
        for i0_ in range(0, 16, 2):
            for i in (i0_, i0_ + 1):
                spdma(xt[i % 2], xtok_d[i * 128:(i + 1) * 128, :], [Bxt[i % 2]])
                zero_fill(48 + i)
                for h in range(2):
                    b = nbank()
                    for kc in range(8):
                        rb_ = [Bwout, BmixC[i // 4]] if kc < 4 else [Bwout, BmixA[i]]
                        mm(pf(b), mixT[:, kc, i * 128:(i + 1) * 128], wout[:, kc, h * 512:(h + 1) * 512], kc == 0, False, rb_, PB[b])
                    mm(pf(b), onesb[0:1, :], boutb[0:1, h * 512:(h + 1) * 512], False, True, [Bconst, Bbout], PB[b])
                    S.op("dve", lambda e, b=b, h=h, i=i: e.scalar_tensor_tensor(out=rbufs[i % 4][:, h * 512:(h + 1) * 512], in0=xt[i % 2][:, h * 512:(h + 1) * 512],
                                                                               scalar=ALPHA, in1=pf(b), op0=ALU.mult, op1=ALU.add),
                         reads=[Bxt[i % 2], PB[b]], writes=[Brbs[i % 4]])
            flush_pend(keep=2)
            lockstep([ln_tile(i, resid[:, i, :], BR[i], transpose_to=True, defer=pend, gain_eng="dve") for i in (i0_, i0_ + 1)])
        flush_pend()
        lnst["nb"] = 2
        S.barrier()
        if stage <= 1:
            for i in range(16):
                spdma(out_d[i * 128:(i + 1) * 128, :], resid[:, i, :], [Buf()], reads=[BR[i]])
            S.finish(); S.flush()
            return nc

        wq = A(0, BF16, 8, 1024); Bwq = Buf("wq")
        wo = A(16384, BF16, 8, 1024); Bwo = Buf("wo")
        bstg = [A(SC2 + 36864 + j * 8192, F32, 2048) for j in range(3)]; Bbstg = [Buf("bstg%d" % j) for j in range(3)]
        bst = {"k": 0}

        def load_w_sp(dst, src2d, wbuf):
            for c in range(4):
                k = bst["k"] % 3
                bst["k"] += 1
                spdma(bstg[k].rearrange("p (a b) -> p a b", a=2), src2d[c * 256:(c + 1) * 256, :].rearrange("(kc p) f -> p kc f", p=128), [Bbstg[k]])
                if bst["k"] % 2 == 0:
                    S.op("dve", lambda e, k=k, c=c: e.tensor_copy(out=dst[:, 2 * c:2 * c + 2, :], in_=bstg[k].rearrange("p (a b) -> p a b", a=2)), reads=[Bbstg[k]], writes=[wbuf])
                else:
                    S.op("act", lambda e, k=k, c=c: e.copy(out=dst[:, 2 * c:2 * c + 2, :], in_=bstg[k].rearrange("p (a b) -> p a b", a=2)), reads=[Bbstg[k]], writes=[wbuf])
        KmT = A(SC2, BF16, 8, 256); BKm = Buf("KmT")
        Vm = A(SC2 + 4096, BF16, 2, 1024); BVm = Buf("Vm")
        wkv = A(SC2 + 8192, BF16, 8, 1024); Bwkv = Buf("wkv")
        memT = A(SC2 + 8192 + 16384, BF16, 8, 256); BmemT = Buf("memT")
        k_ = bst["k"] % 3
        bst["k"] += 1
        spdma(bstg[k_].rearrange("p (a b) -> p a b", a=8), memT_d.rearrange("(kc p) t -> p kc t", p=128), [Bbstg[k_]])
        S.op("dve", lambda e, k_=k_: e.tensor_copy(out=memT, in_=bstg[k_].rearrange("p (a b) -> p a b", a=8)), reads=[Bbstg[k_]], writes=[BmemT])
        load_w_sp(wkv, wkv_d[:, 0:1024], Bwkv)
        for fc in range(8):
            b = nbank()
            for kc in range(8):
                mm(pf(b, 256), wkv[:, kc, fc * 128:(fc + 1) * 128], memT[:, kc, :], kc == 0, kc == 7, [Bwkv, BmemT], PB[b])
            S.op("act", lambda e, b=b, fc=fc: e.copy(out=KmT[:, fc, :], in_=pf(b, 256)), reads=[PB[b]], writes=[BKm])
        load_w_sp(wq, wq_d, Bwq)
        load_w_sp(wkv, wkv_d[:, 1024:2048], Bwkv)
        for mt in range(2):
            for h in range(2):
                b = nbank()
                for kc in range(8):
                    mm(pf(b), memT[:, kc, mt * 128:(mt + 1) * 128], wkv[:, kc, h * 512:(h + 1) * 512], kc == 0, kc == 7, [Bwkv, BmemT], PB[b])
                S.op("act", lambda e, b=b, mt=mt, h=h: e.copy(out=Vm[:, mt, h * 512:(h + 1) * 512], in_=pf(b)), reads=[PB[b]], writes=[BVm])
        load_ln(1)
        load_w_sp(wo, wo_d, Bwo)
        S.barrier()
        x2Tfs = [A(SC2 + 45056, F32, 8, 128), A(SC2 + 40960, F32, 8, 128)]; Bx2Ts = [Buf("x2Tf0"), Buf("x2Tf1")]
        NXR = 6
        xrow = [A(SC2 + 49152 + i * 2304, BF16, 1152) for i in range(NXR)]; Bxrow = [Buf("xrow%d" % i) for i in range(NXR)]
        RTs = [A(SC2 + 62976 + (j % 2) * 2816, F32, 11, 64) for j in range(2)]; Brts = [Buf("rt%d" % j) for j in range(2)]
        selall = A(SC2 + 68608, BF16, 16, 64); Bsel = [Buf("sel%d" % i) for i in range(16)]
        slots = SLOTS; Bslot = [Buf("slot%d" % i) for i in range(16)]
        wr = A(SC2 + 70656, F32, 8, 64); Bwr = Buf("wr")
        spdma(wr, wr_d.rearrange("(kc p) f -> p kc f", p=128), [Bwr])
        rbbc = A(SC2 + 72704, F32, 64); ecol = A(SC2 + 72960, F32, 64); Brc = Buf("rbec")
        spdma(rbbc, rb_d[0:1, :].broadcast_to([128, 64]), [Brc])
        spdma(ecol, ecol_d[0:1, :].broadcast_to([128, 64]), [Brc])
        BIG = 1.0e9
        def c1_tile(i, part):
            RT, Brt = RTs[i % 2], Brts[i % 2]
            x2Tf, Bx2T = x2Tfs[i % 2], Bx2Ts[i % 2]
            sc, ch, eq, c2, w_, mc, key, t1, selc = (RT[:, j, :] for j in range(9))
            g8 = lambda j, RT=RT: RT[:, 9, j * 8:(j + 1) * 8]
            ws1 = RT[:, 10, 0:1]; rs1 = RT[:, 10, 1:2]
            xr, bxr = xrow[i % NXR], Bxrow[i % NXR]
            gwv = xr[:, 1024:1152].bitcast(F32)
            V3 = lambda ap: ap.rearrange("p (a b) -> p a b", a=8)
            R = [Brt]
            if part == "A":
                x2Tf, Bx2T = x2Tfs[i % 2], Bx2Ts[i % 2]
                b0, b1 = nbank(), nbank()
                for kc in range(8):
                    bb = b0 if kc < 4 else b1
                    S.op("pe", lambda e, bb=bb, kc=kc, i=i: e.transpose(out=PS[:, bb, (kc % 4) * 128:(kc % 4 + 1) * 128], in_=resid[:, i, kc * 128:(kc + 1) * 128], identity=identf[:]),
                         reads=[BR[i], Bconst], writes=[PB[bb]])
                    yield
                S.op("act", lambda e, b0=b0: e.copy(out=x2Tf[:, 0:4, :], in_=pf(b0).rearrange("p (a b) -> p a b", a=4)), reads=[PB[b0]], writes=[Bx2T])
                yield
                S.op("dve", lambda e, b1=b1: e.tensor_copy(out=x2Tf[:, 4:8, :], in_=pf(b1).rearrange("p (a b) -> p a b", a=4)), reads=[PB[b1]], writes=[Bx2T])
                yield
                bl = nbank()
                for kc in range(8):
                    mm(pf(bl, 64), x2Tf[:, kc, :], wr[:, kc, :], kc == 0, kc == 7, [Bx2T, Bwr], PB[bl])
                S.op("act", lambda e, bl=bl: e.activation(out=c2, in_=pf(bl, 64), func=AF.Exp, scale=-1.0), reads=[PB[bl]], writes=R)
                yield
                S.op("dve", lambda e: e.tensor_scalar(out=c2, in0=c2, scalar1=1.0, scalar2=None, op0=ALU.add), reads=R, writes=R)
                yield
                S.op("dve", lambda e: e.reciprocal(out=sc, in_=c2), reads=R, writes=R)
                yield
                S.op("dve", lambda e: e.tensor_tensor(out=ch, in0=sc, in1=rbbc, op=ALU.add), reads=R + [Brc], writes=R)
                yield
                S.op("dve", lambda e: e.tensor_reduce(out=g8(0), in_=V3(ch), axis=AX.X, op=ALU.max), reads=R, writes=R)
                yield
                S.op("dve", lambda e: e.tensor_tensor(out=V3(eq), in0=V3(ch), in1=g8(0).unsqueeze(2).broadcast_to([128, 8, 8]), op=ALU.is_equal), reads=R, writes=R)
                yield
                S.op("dve", lambda e: e.scalar_tensor_tensor(out=c2, in0=eq, scalar=-BIG, in1=ch, op0=ALU.mult, op1=ALU.add), reads=R, writes=R)
                yield
                S.op("dve", lambda e: e.tensor_reduce(out=g8(1), in_=V3(c2), axis=AX.X, op=ALU.max), reads=R, writes=R)
                yield
                S.op("dve", lambda e: e.tensor_tensor(out=g8(2), in0=g8(0), in1=g8(1), op=ALU.add), reads=R, writes=R)
                yield
                S.op("dve", lambda e: e.max(out=g8(3), in_=g8(2)), reads=R, writes=R)
                yield
                S.op("dve", lambda e: e.tensor_scalar(out=g8(4), in0=g8(2), scalar1=RT[:, 9, 27:28], scalar2=None, op0=ALU.is_ge), reads=R, writes=R)
                yield
                S.op("dve", lambda e: e.tensor_scalar(out=g8(5), in0=g8(4), scalar1=-1.0, scalar2=BIG, op0=ALU.add, op1=ALU.mult), reads=R, writes=R)
                yield
                S.op("dve", lambda e: e.tensor_tensor(out=V3(mc), in0=V3(ch), in1=g8(5).unsqueeze(2).broadcast_to([128, 8, 8]), op=ALU.add), reads=R, writes=R)
                yield
                S.op("dve", lambda e: e.max(out=g8(6), in_=mc), reads=R, writes=R)
                yield
                S.op("dve", lambda e: e.tensor_scalar(out=eq, in0=mc, scalar1=RT[:, 9, 55:56], scalar2=None, op0=ALU.is_ge), reads=R, writes=R)
                yield
                S.op("dve", lambda e: e.tensor_tensor(out=w_, in0=sc, in1=eq, op=ALU.mult), reads=R, writes=R)
                yield
                S.op("dve", lambda e: e.tensor_reduce(out=ws1, in_=w_, axis=AX.X, op=ALU.add), reads=R, writes=R)
                yield
                S.op("dve", lambda e: e.reciprocal(out=rs1, in_=ws1), reads=R, writes=R)
                yield
                S.op("dve", lambda e, gwv=gwv: e.tensor_scalar(out=gwv, in0=w_, scalar1=rs1, scalar2=2.5, op0=ALU.mult, op1=ALU.mult), reads=R, writes=[bxr])
                yield
                S.op("dve", lambda e, i=i: e.tensor_copy(out=selall[:, i, :], in_=eq), reads=R, writes=[Bsel[i]])
                yield
                return
            bp = nbank()
            for j in range(i + 1):
                mm(pf(bp, 64), trib[:] if j == i else onesb[:], selall[:, j, :], j == 0, j == i, [Bconst, Bsel[j]], PB[bp])
            S.op("dve", lambda e, bp=bp: e.scalar_tensor_tensor(out=selc, in0=pf(bp, 64), scalar=float(C_CAP), in1=eq, op0=ALU.is_lt, op1=ALU.mult), reads=R + [PB[bp]], writes=R)
            yield
            S.op("dve", lambda e, bp=bp: e.tensor_tensor(out=t1, in0=pf(bp, 64), in1=ecol, op=ALU.add), reads=R + [PB[bp], Brc], writes=R)
            yield
            S.op("dve", lambda e: e.scalar_tensor_tensor(out=key, in0=t1, scalar=1.0, in1=selc, op0=ALU.add, op1=ALU.mult), reads=R, writes=R)
            yield
            S.op("dve", lambda e: e.tensor_scalar(out=key, in0=key, scalar1=-1.0, scalar2=None, op0=ALU.add), reads=R, writes=R)
            yield
            S.op("dve", lambda e: e.max(out=g8(7), in_=key), reads=R, writes=R)
            yield
            S.op("dve", lambda e: e.tensor_scalar(out=g8(6), in0=g8(7), scalar1=0.0, scalar2=float(NSLOT + 1), op0=ALU.is_lt, op1=ALU.mult), reads=R, writes=R)
            yield
            S.op("dve", lambda e: e.tensor_tensor(out=g8(7), in0=g8(7), in1=g8(6), op=ALU.add), reads=R, writes=R)
            yield
            S.op("dve", lambda e, i=i: e.tensor_copy(out=slots[:, i, :], in_=g8(7)), reads=R, writes=[Bslot[i]])
            yield
            S.op("act", lambda e, xr=xr, i=i: e.copy(out=xr[:, 0:1024].rearrange("q (k p) -> q p k", k=8), in_=resid[:, i, :].rearrange("q (p k) -> q p k", k=8)), reads=[BR[i]], writes=[bxr])
            yield
            for k in range(8):
                S.dma("pool", lambda e, xr=xr, i=i, k=k: e.indirect_dma_start(
                    out=xg_d, out_offset=bass.IndirectOffsetOnAxis(ap=slots[:, i, k:k + 1], axis=0), in_=xr, in_offset=None), reads=[bxr, Bslot[i], Bxg], writes=[Buf()])
        qTh = [A(SC2 + 8192 + j * 2048, BF16, 2, 512) for j in range(2)]; BqTh = [Buf("qTh0"), Buf("qTh1")]
        oTg = A(SC2 + 12288, BF16, 8, 512); BoT = Buf("oTg")
        Exs = [A(SC2 + 20480 + i * 1024, BF16, 512) for i in range(4)]; BExs = [Buf("Ex%d" % i) for i in range(4)]
        rdxs = [A(SC2 + 24576 + i * 2048, F32, 512) for i in range(2)]; Brdxs = [Buf("rdx0"), Buf("rdx1")]
        for tg in range(4):
            def qproj(h, tg=tg):
                qT_, bqT_ = qTh[h % 2], BqTh[h % 2]
                for j in range(2):
                    fc = 2 * h + j
                    b = nbank()
                    for kc in range(8):
                        mm(pf(b), wq[:, kc, fc * 128:(fc + 1) * 128], xs_[:, kc, tg * 512:(tg + 1) * 512], kc == 0, kc == 7, [Bwq, BX[tg]], PB[b])
                    S.op("act", lambda e, b=b, j=j, qT_=qT_: e.copy(out=qT_[:, j, :], in_=pf(b)), reads=[PB[b]], writes=[bqT_])

            def logits(h):
                qT_, bqT_ = qTh[h % 2], BqTh[h % 2]
                Ex = Exs[(h % 2) * 2:(h % 2) * 2 + 2]; BEx = BExs[(h % 2) * 2:(h % 2) * 2 + 2]
                for mt in range(2):
                    b = nbank()
                    for j in range(2):
                        mm(pf(b), KmT[:, 2 * h + j, mt * 128:(mt + 1) * 128], qT_[:, j, :], j == 0, j == 1, [BKm, bqT_], PB[b])
                    S.op("act", lambda e, b=b, mt=mt, Ex=Ex: e.activation(out=Ex[mt], in_=pf(b), func=AF.Exp, scale=1.0 / 16.0), reads=[PB[b]], writes=[BEx[mt]])

            def denpv(h):
                Ex = Exs[(h % 2) * 2:(h % 2) * 2 + 2]; BEx = BExs[(h % 2) * 2:(h % 2) * 2 + 2]
                rdx, Brdx = rdxs[h % 2], Brdxs[h % 2]
                bd = nbank()
                for mt in range(2):
                    mm(pf(bd), onesb[:], Ex[mt], mt == 0, mt == 1, [Bconst, BEx[mt]], PB[bd])
                S.op("act", lambda e, bd=bd, rdx=rdx: e.activation(out=rdx, in_=pf(bd), func=AF.Ln), reads=[PB[bd]], writes=[Brdx])
                S.op("act", lambda e, rdx=rdx: e.activation(out=rdx, in_=rdx, func=AF.Exp, scale=-1.0), reads=[Brdx], writes=[Brdx])
                for j in range(2):
                    b = nbank()
                    for mt in range(2):
                        mm(pf(b), Vm[:, mt, (2 * h + j) * 128:(2 * h + j + 1) * 128], Ex[mt], mt == 0, mt == 1, [BVm, BEx[mt]], PB[b])
                    S.op("dve", lambda e, b=b, h=h, j=j, rdx=rdx: e.tensor_tensor(out=oTg[:, 2 * h + j, :], in0=pf(b), in1=rdx, op=ALU.mult),
                         reads=[PB[b], Brdx], writes=[BoT])
            qproj(0)
            flush_pend()
            for h in range(4):
                logits(h)
                if h + 1 < 4:
                    qproj(h + 1)
                denpv(h)
            for p0 in (0, 2):
                pair = (tg * 4 + p0, tg * 4 + p0 + 1)
                for i in pair:
                    il = i - tg * 4
                    for h in range(2):
                        b = nbank()
                        for kc in range(8):
                            mm(pf(b), oTg[:, kc, il * 128:(il + 1) * 128], wo[:, kc, h * 512:(h + 1) * 512], kc == 0, kc == 7, [Bwo, BoT], PB[b])
                        S.op("dve", lambda e, b=b, h=h, i=i: e.scalar_tensor_tensor(out=resid[:, i, h * 512:(h + 1) * 512], in0=resid[:, i, h * 512:(h + 1) * 512],
                                                                                   scalar=ALPHA, in1=pf(b), op0=ALU.mult, op1=ALU.add),
                             reads=[BR[i], PB[b]], writes=[BR[i]])
                flush_pend()
                lockstep([ln_tile(i, resid[:, i, :], BR[i], transpose_to=True, defer=pend, gain_eng="dve", src=resid[:, i, :], srcbuf=BR[i]) for i in pair])
                if stage > 2:
                    pk = pair[0] // 2
                    if pk >= 1:
                        lockstep([c1_tile(i, "A") for i in (2 * pk - 2, 2 * pk - 1)])
                        lockstep([c1_tile(i, "B") for i in (2 * pk - 2, 2 * pk - 1)])
        flush_pend()
        if stage > 2:
            lockstep([c1_tile(i, "A") for i in (14, 15)])
            lockstep([c1_tile(i, "B") for i in (14, 15)])
        S.barrier(skip_q=("pool",) if stage > 2 else ())
        if stage <= 2:
            for i in range(16):
                spdma(out_d[i * 128:(i + 1) * 128, :], resid[:, i, :], [Buf()], reads=[BR[i]])
            S.finish(); S.flush()
            return nc

        wsg = A(SC2, BF16, 8, 256); wsu = A(SC2 + 4096, BF16, 8, 256); wsd = A(SC2 + 8192, BF16, 2, 1024); Bws = Buf("ws")
        wstg = [A(SC2 + 32768, F32, 2048), A(SC2 + 40960, F32, 2048)]; Bwstg = [Buf("wstg0"), Buf("wstg1")]
        spdma(wstg[0].rearrange("p (a b) -> p a b", a=8), sg_d.rearrange("(kc p) f -> p kc f", p=128), [Bwstg[0]])
        spdma(wstg[1].rearrange("p (a b) -> p a b", a=8), su_d.rearrange("(kc p) f -> p kc f", p=128), [Bwstg[1]])
        S.op("dve", lambda e: e.tensor_copy(out=wsg.rearrange("p a b -> p (a b)"), in_=wstg[0]), reads=[Bwstg[0]], writes=[Bws])
        S.op("act", lambda e: e.copy(out=wsu.rearrange("p a b -> p (a b)"), in_=wstg[1]), reads=[Bwstg[1]], writes=[Bws])
        spdma(wstg[0].rearrange("p (a b) -> p a b", a=2), sd_d.rearrange("(fc p) d -> p fc d", p=128), [Bwstg[0]])
        S.op("dve", lambda e: e.tensor_copy(out=wsd.rearrange("p a b -> p (a b)"), in_=wstg[0]), reads=[Bwstg[0]], writes=[Bws])
        hsh = A(SC2 + 12288, BF16, 2, 2048); Bhsh = [Buf("hsh%d" % g) for g in range(4)]
        wbase = [0, 12288]
        wgt = [(A(o, BF16, 8, 256), A(o + 4096, BF16, 8, 256), A(o + 8192, BF16, 2, 1024)) for o in wbase]
        Bwgt = [Buf("wgt0"), Buf("wgt1")]
        pstg = A(SC2 + 65536, F32, 2048); Bpstg = Buf("pstg")
        pre_items = [(e_, m) for e_ in range(2) for m in range(3)]
        pre_state = {"k": 0}

        def pre_step():
            k = pre_state["k"]
            pre_state["k"] += 1
            if 1 <= k <= len(pre_items):
                e_, m = pre_items[k - 1]
                wv = wgt[e_][m].rearrange("p a b -> p (a b)")
                if k % 2 == 0:
                    S.op("dve", lambda e, wv=wv: e.tensor_copy(out=wv, in_=pstg), reads=[Bpstg], writes=[Bwgt[e_]])
                else:
                    S.op("act", lambda e, wv=wv: e.copy(out=wv, in_=pstg), reads=[Bpstg], writes=[Bwgt[e_]])
            if k < len(pre_items):
                e_, m = pre_items[k]
                src = (eg_d[e_].rearrange("(p kc) f -> p (kc f)", kc=8), eu_d[e_].rearrange("(p kc) f -> p (kc f)", kc=8),
                       ed_d[e_].rearrange("(fc p) d -> p fc d", p=128))[m]
                dst = pstg if m < 2 else pstg.rearrange("p (a b) -> p a b", a=2)
                spdma(dst, src, [Bpstg])
        silt = [A(SC2 + 30720 + i * 2048, F32, 512) for i in range(1)]; Bsil = [Buf("sil0")]
        for tg in range(4):
            for f in range(2):
                pre_step()
                bg_, bu_ = nbank(), nbank()
                for kc in range(8):
                    mm(pf(bg_), wsg[:, kc, f * 128:(f + 1) * 128], xs_[:, kc, tg * 512:(tg + 1) * 512], kc == 0, kc == 7, [Bws, BX[tg]], PB[bg_])
                for kc in range(8):
                    mm(pf(bu_), wsu[:, kc, f * 128:(f + 1) * 128], xs_[:, kc, tg * 512:(tg + 1) * 512], kc == 0, kc == 7, [Bws, BX[tg]], PB[bu_])
                S.op("act", lambda e, bg_=bg_: e.activation(out=silt[0], in_=pf(bg_), func=AF.Silu), reads=[PB[bg_]], writes=[Bsil[0]])
                S.op("dve", lambda e, bu_=bu_, f=f, tg=tg: e.tensor_tensor(out=hsh[:, f, tg * 512:(tg + 1) * 512], in0=pf(bu_), in1=silt[0], op=ALU.mult),
                     reads=[PB[bu_], Bsil[0]], writes=[Bhsh[tg]])
        def shared_down(i):
            for h in range(2):
                b = nbank()
                for f in range(2):
                    mm(pf(b), hsh[:, f, i * 128:(i + 1) * 128], wsd[:, f, h * 512:(h + 1) * 512], f == 0, f == 1, [Bws, Bhsh[i // 4]], PB[b])
                S.op("dve", lambda e, b=b, h=h, i=i: e.scalar_tensor_tensor(out=resid[:, i, h * 512:(h + 1) * 512], in0=resid[:, i, h * 512:(h + 1) * 512],
                                                                           scalar=ALPHA, in1=pf(b), op0=ALU.mult, op1=ALU.add),
                     reads=[BR[i], PB[b]], writes=[BR[i]])
        for i in range(16):
            shared_down(i)
        while pre_state["k"] <= len(pre_items):
            pre_step()
        S.barrier()

        stg_off = [SC2 + 32768 + j * 8192 for j in range(5)] + [98304 + 18432]
        stg = [[A(stg_off[ss * 3 + m], F32, 2048) for m in range(3)] for ss in range(2)]
        Bstg = [[Buf("stg%d_%d" % (ss, m)) for m in range(3)] for ss in range(2)]
        hT = [A(24576 + i * 2048, BF16, 2, C_CAP) for i in range(2)]; BhT = [Buf("hT0"), Buf("hT1")]
        sile = [A(28672 + i * 2048, F32, C_CAP) for i in range(2)]; Bsile = [Buf("sile0"), Buf("sile1")]
        xsl = [A(98304 + i * 9216, BF16, NST, 1152) for i in range(2)]; Bxsl = [Buf("xsl%d" % i) for i in range(2)]
        xgT = [A(SC2 + 16384 + i * 8192, BF16, 8, C_CAP) for i in range(2)]; BxgT = [[Buf("xgT%d_%d" % (i, j)) for j in range(2 * NST)] for i in range(2)]
        yE = [A(SC2 + i * 8192, BF16, NST, 1024) for i in range(2)]; ByE = [[Buf("yE%d_%d" % (i, j)) for j in range(2 * NST)] for i in range(2)]
        Bye_d = Buf("ye_d")

        def load_dma(e_):
            srcs = (eg_d[e_].rearrange("(p kc) f -> p (kc f)", kc=8), eu_d[e_].rearrange("(p kc) f -> p (kc f)", kc=8),
                    ed_d[e_].rearrange("(fc p) d -> p fc d", p=128))
            for m in (1, 2, 0):
                sb = stg[e_ % 2][m]
                dst = sb if m < 2 else sb.rearrange("p (a b) -> p a b", a=2)
                spdma(dst, srcs[m], [Bstg[e_ % 2][m]])

        def load_x(e_):
            S.dma("act", lambda e, e_=e_: e.dma_start(out=xsl[e_ % 2], in_=xg_d[e_ * C_CAP:(e_ + 1) * C_CAP, :].rearrange("(s p) f -> p s f", p=128)),
                  reads=[Bxg], writes=[Bxsl[e_ % 2]])

        def load_cast(e_):
            for m, eng in ((1, "dve"), (2, "act"), (0, "pool")):
                wv = wgt[e_ % 2][m].rearrange("p a b -> p (a b)")
                sb = stg[e_ % 2][m]
                if eng == "act":
                    S.op("act", lambda e, wv=wv, sb=sb: e.copy(out=wv, in_=sb), reads=[Bstg[e_ % 2][m]], writes=[Bwgt[e_ % 2]])
                else:
                    S.op(eng, lambda e, wv=wv, sb=sb: e.tensor_copy(out=wv, in_=sb), reads=[Bstg[e_ % 2][m]], writes=[Bwgt[e_ % 2]])

        NEXP = 64
        load_x(0); load_x(1); load_dma(2); load_dma(3)
        for e_ in range(NEXP):
            pp = e_ % 2
            wg_, wu_, wd_ = wgt[e_ % 2]
            Bw_ = Bwgt[e_ % 2]
            for s in range(NST):
                for hb in range(2):
                    b = nbank()
                    for j in range(4):
                        kc = hb * 4 + j
                        mm(PS[:, b, j * 128:(j + 1) * 128], xsl[e_ % 2][:, s, kc * 128:(kc + 1) * 128], identb[:], True, True, [Bxsl[e_ % 2], Bconst], PB[b])
                    if (s * 2 + hb) % 2 == 0:
                        S.op("dve", lambda e, b=b, s=s, hb=hb, pp=pp: e.tensor_copy(out=xgT[pp][:, hb * 4:hb * 4 + 4, s * 128:(s + 1) * 128], in_=pf(b).rearrange("p (a b) -> p a b", a=4)),
                             reads=[PB[b]], writes=[BxgT[pp][s * 2 + hb]])
                    else:
                        S.op("act", lambda e, b=b, s=s, hb=hb, pp=pp: e.copy(out=xgT[pp][:, hb * 4:hb * 4 + 4, s * 128:(s + 1) * 128], in_=pf(b).rearrange("p (a b) -> p a b", a=4)),
                             reads=[PB[b]], writes=[BxgT[pp][s * 2 + hb]])
            for f in range(2):
                bg_, bu_ = nbank(), nbank()
                for kc in range(8):
                    mm(pf(bg_, C_CAP), wg_[:, kc, f * 128:(f + 1) * 128], xgT[pp][:, kc, :], kc == 0, kc == 7, [Bw_] + BxgT[pp], PB[bg_])
                for kc in range(8):
                    mm(pf(bu_, C_CAP), wu_[:, kc, f * 128:(f + 1) * 128], xgT[pp][:, kc, :], kc == 0, kc == 7, [Bw_] + BxgT[pp], PB[bu_])
                S.op("act", lambda e, bg_=bg_, f=f: e.activation(out=sile[f], in_=pf(bg_, C_CAP), func=AF.Silu), reads=[PB[bg_]], writes=[Bsile[f]])
                S.op("dve", lambda e, bu_=bu_, f=f, pp=pp: e.tensor_tensor(out=hT[pp][:, f, :], in0=pf(bu_, C_CAP), in1=sile[f], op=ALU.mult),
                     reads=[PB[bu_], Bsile[f]], writes=[BhT[pp]])
            for s in range(NST):
                gws = xsl[e_ % 2][:, s, 1024 + 2 * e_:1024 + 2 * e_ + 2].bitcast(F32)
                for h in range(2):
                    b = nbank()
                    for f in range(2):
                        mm(pf(b), hT[pp][:, f, s * 128:(s + 1) * 128], wd_[:, f, h * 512:(h + 1) * 512], f == 0, f == 1, [Bw_, BhT[pp]], PB[b])
                    if h == 0:
                        S.op("act", lambda e, b=b, s=s, h=h, pp=pp, gws=gws: e.activation(out=yE[pp][:, s, h * 512:(h + 1) * 512], in_=pf(b), func=AF.Identity, scale=gws),
                             reads=[PB[b], Bxsl[e_ % 2]], writes=[ByE[pp][s * 2 + h]])
                    else:
                        S.op("dve", lambda e, b=b, s=s, h=h, pp=pp, gws=gws: e.tensor_scalar(out=yE[pp][:, s, h * 512:(h + 1) * 512], in0=pf(b), scalar1=gws, scalar2=None, op0=ALU.mult),
                             reads=[PB[b], Bxsl[e_ % 2]], writes=[ByE[pp][s * 2 + h]])
            S.dma("act", lambda e, e_=e_, pp=pp: e.dma_start(out=ye_d[e_ * C_CAP:(e_ + 1) * C_CAP, :].rearrange("(s p) d -> p s d", p=128), in_=yE[pp]),
                  reads=ByE[pp], writes=[Bye_d])
            if e_ + 2 < NEXP:
                load_x(e_ + 2)
                load_cast(e_ + 2)
            if e_ + 4 < NEXP:
                load_dma(e_ + 4)
        S.barrier()

        gb = [[A(98304 + pp * 16384 + k * 2048, BF16, 1024) for k in range(8)] for pp in range(2)]
        Bgb = [[Buf("gb%d_%d" % (pp, k)) for k in range(8)] for pp in range(2)]
        sm = [A(SC2 + j * 4096, F32, 1024) for j in range(4)]; Bsm2 = [Buf("sm%d" % j) for j in range(4)]
        otiles = [A(SC2 + 16384 + j * 4096, F32, 1024) for j in range(2)]; Bots = [Buf("otile0"), Buf("otile1")]
        load_ln(2)
        spdma(ye_d[NSLOT:NSLOT + 1, :], ZT[0:1, 0:1024], [Bye_d], reads=[Bzt])
        Bout = Buf("out")
        lnst["nb"] = 4

        def c3_pre(i):
            pp = i % 2
            for k in range(8):
                S.dma("pool", lambda e, pp=pp, k=k, i=i: e.indirect_dma_start(
                    out=gb[pp][k], out_offset=None, in_=ye_d, in_offset=bass.IndirectOffsetOnAxis(ap=slots[:, i, k:k + 1], axis=0)), reads=[Bye_d, Bslot[i]], writes=[Bgb[pp][k]])
            for h in range(2):
                b = nbank()
                for k in range(8):
                    mm(pf(b), identb[:], gb[pp][k][:, h * 512:(h + 1) * 512], k == 0, k == 7, [Bconst, Bgb[pp][k]], PB[b])
                S.op("dve", lambda e, b=b, h=h, i=i: e.tensor_tensor(out=rbufs[i % 4][:, h * 512:(h + 1) * 512], in0=resid[:, i, h * 512:(h + 1) * 512], in1=pf(b), op=ALU.add),
                     reads=[BR[i], PB[b]], writes=[Brbs[i % 4]])

        for i0_ in range(0, 16, 2):
            c3_pre(i0_); c3_pre(i0_ + 1)
            lockstep([ln_tile(i, otiles[i % 2], Bots[i % 2], transpose_to=None, gain_eng="dve") for i in (i0_, i0_ + 1)])
            for i in (i0_, i0_ + 1):
                spdma(out_d[i * 128:(i + 1) * 128, :], otiles[i % 2], [Buf()], reads=[Bots[i % 2]])
        S.finish()
        S.flush()
    return nc


def _bucket_table():
    d = np.arange(128)
    n = np.maximum(d, 0)
    exact = 16
    large = exact + (np.log(np.maximum(n, 1).astype(np.float32) / exact) / np.float32(np.log(128 / exact)) * (32 - exact)).astype(np.int32)
    large = np.minimum(large, 31)
    return np.where(n < exact, n, large)


_NC_CACHE = {}


def kernel(x, mem, w_in, b_in, conv_w, conv_b, conv_ln_g, conv_ln_b, attn_sinks, rel_bias,
           w_out, b_out, ln1_g, ln1_b, xq_w, xkv_w, xo_w, ln2_g, ln2_b, router_w, router_b,
           exp_gate, exp_up, exp_down, sh_gate, sh_up, sh_down, ln3_g, ln3_b, _stage=99):
    f32 = np.float32
    x = np.asarray(x, f32); mem = np.asarray(mem, f32)
    w_in = np.asarray(w_in, f32)[0]; b_in = np.asarray(b_in, f32)[0]
    qcols = []
    for c in range(4):
        qcols += list(range(1024 + c * 64, 1024 + (c + 1) * 64)) + list(range(1024 + (4 + c) * 64, 1024 + (5 + c) * 64))
    kcols = list(range(1536, 1664))
    v0 = list(range(1664, 1728)); v1 = list(range(1728, 1792))
    cols = list(range(0, 1024)) + qcols + kcols + v0 + v0 + v1 + v1
    win = np.ascontiguousarray(w_in[:, cols])
    bperm = b_in[cols]
    bcols = np.zeros((128, 13), f32)
    for fc in range(4):
        bcols[:, fc] = bperm[fc * 128:(fc + 1) * 128]
        bcols[:, 4 + fc] = bperm[512 + fc * 128:512 + (fc + 1) * 128]
        bcols[:, 8 + fc] = bperm[1024 + fc * 128:1024 + (fc + 1) * 128]
    bcols[:, 12] = bperm[1536:1664]
    bv = np.ascontiguousarray(bperm[1664:1920][None, :])
    cwm = np.asarray(conv_w, f32)[0]
    cw = np.ascontiguousarray(cwm.T.reshape(4, 128, 31).transpose(1, 0, 2).reshape(128, 124))
    cvec = np.zeros((128, 12), f32)
    for j, v in enumerate((conv_b, conv_ln_g, conv_ln_b)):
        cvec[:, 4 * j:4 * j + 4] = np.asarray(v, f32)[0].reshape(4, 128).T
    lnp = np.ascontiguousarray(np.stack([np.asarray(v, f32)[0] for v in (ln1_g, ln1_b, ln2_g, ln2_b, ln3_g, ln3_b)]))
    bk = _bucket_table()
    ohd = np.zeros((32, 128), f32); ohd[bk, np.arange(128)] = 1.0
    ident = np.eye(128, dtype=f32); Jm = np.ascontiguousarray(ident[::-1])
    tri = np.triu(np.ones((128, 128), f32), 1)
    ecol = (np.arange(64, dtype=f32) * C_CAP)[None, :]
    common = dict(
        win=win, bcols=bcols, bv=bv, cw=cw, cvec=cvec, sinks=np.asarray(attn_sinks, f32).reshape(1, 8),
        relb=np.ascontiguousarray(np.asarray(rel_bias, f32)), wout=np.ascontiguousarray(np.asarray(w_out, f32)[0]),
        bout=np.asarray(b_out, f32).reshape(1, 1024), lnp=lnp,
        wq=np.ascontiguousarray(np.asarray(xq_w, f32)[0]), wkv=np.ascontiguousarray(np.asarray(xkv_w, f32)[0]),
        wo=np.ascontiguousarray(np.asarray(xo_w, f32)[0]), wr=np.ascontiguousarray(np.asarray(router_w, f32)[0]),
        rb=np.asarray(router_b, f32).reshape(1, 64),
        eg=np.ascontiguousarray(np.asarray(exp_gate, f32)[0]), eu=np.ascontiguousarray(np.asarray(exp_up, f32)[0]),
        ed=np.ascontiguousarray(np.asarray(exp_down, f32)[0]),
        sg=np.ascontiguousarray(np.asarray(sh_gate, f32)[0]), su=np.ascontiguousarray(np.asarray(sh_up, f32)[0]),
        sd=np.ascontiguousarray(np.asarray(sh_down, f32)[0]),
        ident=ident, Jm=Jm, ohd=ohd, tri=tri, ecol=ecol)
    in_maps = []
    for c in range(8):
        b, half = c // 2, c % 2
        t0 = half * 2048
        xt = np.zeros((1024, 2176), f32)
        xt[:, 128:] = x[b, t0:t0 + 2048].T
        if half == 1:
            xt[:, :128] = x[b, t0 - 128:t0].T
        m = dict(common)
        xtl = np.zeros((128, 8 * 2176), f32)
        for c0_, n_ in [(0, 128)] + [(128 + g_ * 512, 512) for g_ in range(4)]:
            xtl[:, 8 * c0_:8 * (c0_ + n_)] = xt[:, c0_:c0_ + n_].reshape(8, 128, n_).transpose(1, 0, 2).reshape(128, 8 * n_)
        xt = xtl
        m.update(xT=xt, xtok=np.ascontiguousarray(x[b, t0:t0 + 2048]), memT=np.ascontiguousarray(mem[b].T),
                 hv=np.full((128, 1), float(half), f32))
        in_maps.append(m)
    if _stage not in _NC_CACHE:
        _NC_CACHE[_stage] = build_program(_stage)
    nc = _NC_CACHE[_stage]
    res = run_bass_kernel_spmd(nc, in_maps, core_ids=list(range(8)))
    out = np.zeros((4, 4096, 1024), f32)
    for c in range(8):
        b, half = c // 2, c % 2
        out[b, half * 2048:(half + 1) * 2048] = res.results[c]["out"]
    return out
```
